# Optimizing a Trainium2 kernel written in Bass

```python
import math
import jax, jax.numpy as jnp
from jax import lax
import numpy as np

D_MODEL = 1024
BATCH = 16
SEQ = 4096
DEPTH = 2

D_CONV = D_MODEL
D_HYENA = D_MODEL
N_BRANCHES = 2
CONV_WIDTH = 3
HYENA_ORDER = 2
N_DIRECTIONS = 2
POS_BANDS = 16
POS_EMB_DIM = 1 + 2 * POS_BANDS
FILTER_HIDDEN = 64
DECAY_TARGET = 1e-2
FAST_DECAY_PCT = 0.3
SLOW_DECAY_PCT = 1.5
PROJ_COLS = 3 * D_CONV + (HYENA_ORDER + 1) * D_HYENA + N_BRANCHES * D_MODEL
D_FF = 2816
N_EXPERTS = 8
TOP_K = 2
D_FF_EXPERT = 3584
N_DENSE = (DEPTH + 1) // 2
N_MOE = DEPTH // 2
DEEPNORM_ALPHA = (2 * DEPTH) ** 0.25
DEEPNORM_BETA = (8 * DEPTH) ** -0.25
LN_EPS = 1e-5

kernel_name = "hybrid_shortconv_hyena_moe_deepnorm_encoder"


def layer_norm(x, g, b):
    xf = x.astype(jnp.float32)
    mu = jnp.mean(xf, axis=-1, keepdims=True)
    var = jnp.mean(jnp.square(xf - mu), axis=-1, keepdims=True)
    return ((xf - mu) * lax.rsqrt(var + LN_EPS)).astype(x.dtype) * g + b


def dwconv3_centred(u, w):
    up = jnp.pad(u, ((0, 0), (1, 1), (0, 0)))
    return up[:, :-2] * w[0] + up[:, 1:-1] * w[1] + up[:, 2:] * w[2]


def hyena_pos_features(L):
    t = jnp.linspace(0.0, 1.0, L, dtype=jnp.float32)[:, None]
    bands = jnp.linspace(1e-4, POS_BANDS - 1, POS_BANDS, dtype=jnp.float32)[None, :]
    w = (2.0 * math.pi / L) * jnp.arange(L, dtype=jnp.float32)[:, None]
    ang = bands * w
    return jnp.concatenate([t, jnp.cos(ang), -jnp.sin(ang)], axis=-1), t


def decay_window(t):
    max_decay = math.log(DECAY_TARGET) / FAST_DECAY_PCT
    min_decay = math.log(DECAY_TARGET) / SLOW_DECAY_PCT
    deltas = jnp.linspace(min_decay, max_decay, D_HYENA, dtype=jnp.float32)
    return jnp.exp(-t * jnp.abs(deltas)[None, :])


def implicit_filters(L, w1, b1, freq, w2, b2, w3):
    f32 = jnp.float32
    z, t = hyena_pos_features(L)
    fr = freq.astype(f32)
    h = jnp.sin(fr * (z @ w1.astype(f32) + b1.astype(f32)))
    h = jnp.sin(fr * (h @ w2.astype(f32) + b2.astype(f32)))
    h = (h @ w3.astype(f32)).reshape(L, N_DIRECTIONS, D_HYENA)
    h = h * decay_window(t)[:, None, :]
    h = h * lax.rsqrt(jnp.sum(jnp.square(h), axis=0, keepdims=True) + 1e-6)
    return h[:, 0], h[:, 1]


def bidirectional_fft_conv(u, k_fwd, k_bwd):
    L = u.shape[1]
    n = 2 * L
    kern = jnp.concatenate([k_fwd, jnp.zeros_like(k_fwd[:1]), k_bwd[:0:-1]], axis=0)
    kf = jnp.fft.rfft(kern, n=n, axis=0)
    uf = jnp.fft.rfft(u.astype(jnp.float32), n=n, axis=1)
    y = jnp.fft.irfft(uf * kf[None], n=n, axis=1)[:, :L]
    return y.astype(u.dtype)


def hybrid_mixer(x, w_in, conv_a_w, conv_h_w, conv_h_b, flt_w1, flt_b1, flt_freq, flt_w2, flt_b2, flt_w3,
                 hyena_bias, w_a_out, w_h_out, w_o):
    proj = jnp.einsum('bsd,dp->bsp', x, w_in)
    a_b, a_c, a_u, h_in, gate_logits = jnp.split(
        proj, [D_CONV, 2 * D_CONV, 3 * D_CONV, 3 * D_CONV + (HYENA_ORDER + 1) * D_HYENA], axis=-1)
    y_a = a_b * dwconv3_centred(a_c * a_u, conv_a_w)
    h_in = dwconv3_centred(h_in, conv_h_w) + conv_h_b
    h_v, h_x1, h_x0 = jnp.split(h_in, HYENA_ORDER + 1, axis=-1)
    k_fwd, k_bwd = implicit_filters(x.shape[1], flt_w1, flt_b1, flt_freq, flt_w2, flt_b2, flt_w3)
    z = h_v * h_x1
    z = bidirectional_fft_conv(z, k_fwd, k_bwd) + z * hyena_bias
    y_h = h_x0 * z
    g_a, g_h = jnp.split(jax.nn.sigmoid(gate_logits), N_BRANCHES, axis=-1)
    merged = (g_a * jnp.einsum('bsc,cd->bsd', y_a, w_a_out)
              + g_h * jnp.einsum('bsc,cd->bsd', y_h, w_h_out))
    return jnp.einsum('bsd,de->bse', merged, w_o)


def swiglu(x, w1, w3, w2):
    return (jax.nn.silu(x @ w1) * (x @ w3)) @ w2


def moe_swiglu(x, router, w1, w3, w2):
    bsz, seq, d = x.shape
    xt = x.reshape(bsz * seq, d)
    logits = (xt @ router).astype(jnp.float32)
    top_vals, top_idx = lax.top_k(logits, TOP_K)
    top_w = jax.nn.softmax(top_vals, axis=-1)
    combine = jnp.einsum('tk,tke->te', top_w,
                         jax.nn.one_hot(top_idx, N_EXPERTS, dtype=jnp.float32)).astype(x.dtype)
    y = jnp.zeros_like(xt)
    for e in range(N_EXPERTS):
        y = y + combine[:, e:e + 1] * swiglu(xt, w1[e], w3[e], w2[e])
    return y.reshape(bsz, seq, d)


def setup_inputs(seed: int = 0) -> dict:
    key = jax.random.key(seed)
    ks = iter(jax.random.split(key, 32))

    def nrm(shape, scale):
        return jax.random.normal(next(ks), shape, jnp.float32) * scale

    D = D_MODEL
    return {
        "x": nrm((BATCH, SEQ, D), 1.0),
        "ln_in_g": 1.0 + nrm((D,), 0.01),
        "ln_in_b": nrm((D,), 0.01),
        "w_in": nrm((DEPTH, D, PROJ_COLS), D ** -0.5),
        "conv_a_w": nrm((DEPTH, CONV_WIDTH, D_CONV), CONV_WIDTH ** -0.5),
        "conv_h_w": nrm((DEPTH, CONV_WIDTH, (HYENA_ORDER + 1) * D_HYENA), CONV_WIDTH ** -0.5),
        "conv_h_b": nrm((DEPTH, (HYENA_ORDER + 1) * D_HYENA), 0.01),
        "flt_w1": nrm((DEPTH, POS_EMB_DIM, FILTER_HIDDEN), POS_EMB_DIM ** -0.5),
        "flt_b1": nrm((DEPTH, FILTER_HIDDEN), 0.01),
        "flt_freq": 1.0 + nrm((DEPTH, FILTER_HIDDEN), 0.01),
        "flt_w2": nrm((DEPTH, FILTER_HIDDEN, FILTER_HIDDEN), FILTER_HIDDEN ** -0.5),
        "flt_b2": nrm((DEPTH, FILTER_HIDDEN), 0.01),
        "flt_w3": nrm((DEPTH, FILTER_HIDDEN, N_DIRECTIONS * D_HYENA), FILTER_HIDDEN ** -0.5),
        "hyena_bias": nrm((DEPTH, D_HYENA), 1.0),
        "w_a_out": nrm((DEPTH, D_CONV, D), D_CONV ** -0.5),
        "w_h_out": nrm((DEPTH, D_HYENA, D), D_HYENA ** -0.5),
        "w_o": nrm((DEPTH, D, D), DEEPNORM_BETA * D ** -0.5),
        "ln_mix_g": 1.0 + nrm((DEPTH, D), 0.01),
        "ln_mix_b": nrm((DEPTH, D), 0.01),
        "ffn_w1": nrm((N_DENSE, D, D_FF), D ** -0.5),
        "ffn_w3": nrm((N_DENSE, D, D_FF), D ** -0.5),
        "ffn_w2": nrm((N_DENSE, D_FF, D), DEEPNORM_BETA * D_FF ** -0.5),
        "moe_router": nrm((N_MOE, D, N_EXPERTS), D ** -0.5),
        "moe_w1": nrm((N_MOE, N_EXPERTS, D, D_FF_EXPERT), D ** -0.5),
        "moe_w3": nrm((N_MOE, N_EXPERTS, D, D_FF_EXPERT), D ** -0.5),
        "moe_w2": nrm((N_MOE, N_EXPERTS, D_FF_EXPERT, D), DEEPNORM_BETA * D_FF_EXPERT ** -0.5),
        "ln_ffn_g": 1.0 + nrm((DEPTH, D), 0.01),
        "ln_ffn_b": nrm((DEPTH, D), 0.01),
    }


def reference(x, ln_in_g, ln_in_b, w_in, conv_a_w, conv_h_w, conv_h_b, flt_w1, flt_b1, flt_freq, flt_w2, flt_b2,
              flt_w3, hyena_bias, w_a_out, w_h_out, w_o, ln_mix_g, ln_mix_b, ffn_w1, ffn_w3, ffn_w2, moe_router,
              moe_w1, moe_w3, moe_w2, ln_ffn_g, ln_ffn_b):
    h = layer_norm(x, ln_in_g, ln_in_b)
    for l in range(DEPTH):
        mix = hybrid_mixer(h, w_in[l], conv_a_w[l], conv_h_w[l], conv_h_b[l], flt_w1[l], flt_b1[l], flt_freq[l],
                           flt_w2[l], flt_b2[l], flt_w3[l], hyena_bias[l], w_a_out[l], w_h_out[l], w_o[l])
        h = layer_norm(DEEPNORM_ALPHA * h + mix, ln_mix_g[l], ln_mix_b[l])
        j = l // 2
        if l % 2 == 0:
            f = swiglu(h, ffn_w1[j], ffn_w3[j], ffn_w2[j])
        else:
            f = moe_swiglu(h, moe_router[j], moe_w1[j], moe_w3[j], moe_w2[j])
        h = layer_norm(DEEPNORM_ALPHA * h + f, ln_ffn_g[l], ln_ffn_b[l])
    return h
```

```python
import math
from contextlib import ExitStack

import numpy as np
import ml_dtypes
import concourse.bass as bass
import concourse.mybir as mybir
from concourse.bass_utils import run_bass_kernel_spmd

F32 = mybir.dt.float32
BF16 = mybir.dt.bfloat16
AF = mybir.ActivationFunctionType
ALU = mybir.AluOpType

NCORES = 8
D = 1024
L = 4096
NBC = 2
T = NBC * L
NFFT = 2 * L
DFF = 2816
DFE = 3584
NE = 8
ALPHA = (2 * 2) ** 0.25
LN_EPS = 1e-5
PI = math.pi


class Buf:
    __slots__ = ("w", "r", "multi")

    def __init__(self, multi=False):
        self.w = []
        self.r = []
        self.multi = multi


class Phase:
    ENG = ("pe", "act", "dve", "pool", "sp")

    def __init__(self, nc, name):
        self.nc = nc
        self.name = name
        self.es = ExitStack()
        self.ops = {e: [] for e in self.ENG}
        pool = getattr(nc, "_mk_sempool", None)
        if pool is None:
            pool = {"eng": {e: nc.alloc_semaphore(name=f"mk_{e}") for e in self.ENG},
                    "cnt": {e: 0 for e in self.ENG}, "dma": []}
            nc._mk_sempool = pool
        self.pool = pool
        self.sem = pool["eng"]
        self.cnt = pool["cnt"]
        self.dslot = {}
        self.waited = {e: {} for e in self.ENG}
        self.nal = 0

    def sb(self, shape, dt):
        self.nal += 1
        return self.es.enter_context(self.nc.sbuf_tensor(f"{self.name}_sb{self.nal}", list(shape), dt))

    def ps(self, shape, dt=F32):
        self.nal += 1
        return self.es.enter_context(self.nc.psum_tensor(f"{self.name}_ps{self.nal}", list(shape), dt))

    def _waits(self, eng, evs):
        best = {}
        for ev in evs:
            if ev is None:
                continue
            sem, val, key = ev
            if self.waited[eng].get(key, 0) >= val:
                continue
            if key not in best or best[key][1] < val:
                best[key] = (sem, val)
        out = []
        for key, (sem, val) in best.items():
            self.waited[eng][key] = val
            out.append((sem, val))
        return out

    def op(self, eng, fn, reads=(), writes=()):
        evs = []
        for b in reads:
            evs.extend(b.w)
        for b in writes:
            evs.extend(b.w)
            evs.extend(b.r)
        waits = self._waits(eng, evs)
        self.cnt[eng] += 1
        ev = (self.sem[eng], self.cnt[eng], eng)
        self.ops[eng].append((waits, fn, (self.sem[eng], 1)))
        for b in reads:
            b.r.append(ev)
        for b in writes:
            b.w = [ev]
            b.r = []
        return ev

    def dma(self, q, out, in_, reads=(), writes=(), key=None):
        kid = id(key)
        if kid not in self.dslot:
            i = len(self.dslot)
            if i >= len(self.pool["dma"]):
                self.pool["dma"].append([self.nc.alloc_semaphore(name=f"mk_d{i}"), 0])
            self.dslot[kid] = self.pool["dma"][i]
        slot = self.dslot[kid]
        sem = slot[0]
        dk = ("d", kid)
        evs = []
        for b in reads:
            evs.extend(b.w)
        for b in writes:
            if not b.multi:
                evs.extend(w for w in b.w if w[2] != dk)
            evs.extend(b.r)
        waits = self._waits(q, evs)
        slot[1] += 16
        ev = (sem, slot[1], dk)
        self.ops[q].append((waits, (lambda e, o=out, i=in_: e.dma_start(out=o, in_=i)), (sem, 16)))
        for b in reads:
            b.r.append(ev)
        for b in writes:
            if b.multi:
                b.w = [w for w in b.w if w[2] != dk] + [ev]
            else:
                b.w = [ev]
            b.r = []
        return ev

    def finish(self):
        nc = self.nc
        final = [(sl[0], sl[1]) for sl in self.dslot.values()]
        final += [(self.sem[e], self.cnt[e]) for e in self.ENG if self.cnt[e] > 0]
        ops = self.ops

        def replay(e, lst):
            for waits, fn, inc in lst:
                for sem, val in waits:
                    e.wait_ge(sem, val)
                fn(e).then_inc(inc[0], inc[1])

        with nc.Block() as block:
            @block.sync
            def _(e):
                replay(e, ops["sp"])
                for sem, val in final:
                    e.wait_ge(sem, val)

            @block.tensor
            def _(e):
                replay(e, ops["pe"])

            @block.scalar
            def _(e):
                replay(e, ops["act"])

            @block.vector
            def _(e):
                replay(e, ops["dve"])

            @block.gpsimd
            def _(e):
                replay(e, ops["pool"])
        self.es.close()


class Ring:
    def __init__(self, P, shape, dt, n, psum=False):
        self.t = [(P.ps(shape, dt) if psum else P.sb(shape, dt)) for _ in range(n)]
        self.b = [Buf() for _ in range(n)]
        self.i = 0

    def next(self):
        k = self.i % len(self.t)
        self.i += 1
        return self.t[k], self.b[k]


def load_const(P, shape, dt, src, q="sp"):
    t = P.sb(shape, dt)
    b = Buf()
    P.dma(q, t[:], src, writes=[b], key=b)
    return t, b


class LNCtx:
    def __init__(self, P, G, gam_row, bet_row, want_T=True, tp_ring=None):
        self.P = P
        self.gam, self.gamB = load_const(P, [128, D], F32, gam_row.partition_broadcast(128))
        self.bet, self.betB = load_const(P, [128, D], F32, bet_row.partition_broadcast(128))
        self.eps, self.epsB = load_const(P, [128, 1], F32, G["eps"])
        self.st = Ring(P, [128, 12], F32, 2)
        self.mv = Ring(P, [128, 4], F32, 2)
        self.want_T = want_T
        if want_T:
            self.idb, self.idbB = load_const(P, [128, 128], BF16, G["identb"])
            self.hb = Ring(P, [128, D], BF16, 2)
            self.tp = tp_ring if tp_ring is not None else Ring(P, [128, 8, 128], BF16, 2, psum=True)
            self.stage = Ring(P, [128, 8, 512], BF16, 2)

    def norm(self, r, rB, hn, hnB):
        P = self.P
        st, stB = self.st.next()
        mv, mvB = self.mv.next()

        def f1(e):
            e.bn_stats(st[:, 0:6], r[:, 0:512])
            return e.bn_stats(st[:, 6:12], r[:, 512:1024])
        P.op("dve", f1, reads=[rB], writes=[stB])
        P.op("dve", lambda e: e.bn_aggr(mv[:, 0:2], st[:, 0:12]), reads=[stB], writes=[mvB])
        P.op("act", lambda e: e.activation(mv[:, 2:3], mv[:, 1:2], AF.Sqrt, bias=self.eps[:, 0:1], scale=1.0),
             reads=[mvB, self.epsB], writes=[mvB])
        P.op("dve", lambda e: e.reciprocal(mv[:, 2:3], mv[:, 2:3]), reads=[mvB], writes=[mvB])
        P.op("dve", lambda e: e.scalar_tensor_tensor(out=mv[:, 3:4], in0=mv[:, 0:1], scalar=-1.0, in1=mv[:, 2:3],
                                                     op0=ALU.mult, op1=ALU.mult), reads=[mvB], writes=[mvB])
        P.op("act", lambda e: e.activation(hn[:], r[:], AF.Identity, bias=mv[:, 3:4], scale=mv[:, 2:3]),
             reads=[rB, mvB], writes=[hnB])
        P.op("dve", lambda e: e.tensor_tensor(out=hn[:], in0=hn[:], in1=self.gam[:], op=ALU.mult),
             reads=[hnB, self.gamB], writes=[hnB])
        P.op("pool", lambda e: e.tensor_tensor(out=hn[:], in0=hn[:], in1=self.bet[:], op=ALU.add),
             reads=[hnB, self.betB], writes=[hnB])

    def transpose_into(self, hn, hnB, stage, stageB, i):
        P = self.P
        hb, hbB = self.hb.next()
        tp, tpB = self.tp.next()
        P.op("act", lambda e: e.copy(hb[:], hn[:]), reads=[hnB], writes=[hbB])

        def ft(e):
            for j in range(8):
                ins = e.transpose(tp[:, j, :], hb[:, j * 128:(j + 1) * 128], self.idb[:])
            return ins
        P.op("pe", ft, reads=[hbB, self.idbB], writes=[tpB])
        P.op("dve", lambda e: e.tensor_copy(stage[:, :, i * 128:(i + 1) * 128], tp[:, :, :]),
             reads=[tpB], writes=[stageB])


def ht_view(G):
    return G["HT"].rearrange("(kc p) t -> p kc t", p=128)


def hts_view(G):
    return G["HTsrc"].rearrange("(kc p) t -> p kc t", p=128)


def phase_ln0(nc, G):
    P = Phase(nc, "ln0")
    C = LNCtx(P, G, G["ln_in_g"], G["ln_in_b"])
    xin = Ring(P, [128, D], F32, 3)
    hnr = Ring(P, [128, D], F32, 3)
    HTv = ht_view(G)
    dH = Buf(multi=True)
    dHT = Buf(multi=True)
    for g4 in range(T // 512):
        stage, stageB = C.stage.next()
        for i in range(4):
            t0 = (g4 * 4 + i) * 128
            r, rB = xin.next()
            hn, hnB = hnr.next()
            P.dma("sp", r[:], G["x"][t0:t0 + 128, :], writes=[rB], key=rB)
            C.norm(r, rB, hn, hnB)
            P.dma("pool", G["H"][t0:t0 + 128, :], hn[:], reads=[hnB], writes=[dH], key=hnB)
            C.transpose_into(hn, hnB, stage, stageB, i)
        P.dma("pool", HTv[:, :, g4 * 512:(g4 + 1) * 512], stage[:], reads=[stageB], writes=[dHT], key=stageB)
    P.finish()


def phase_a(nc, G, l):
    P = Phase(nc, f"a{l}")
    wv = G["w_in"][l].rearrange("(kc p) n -> p kc n", p=128)
    HTv = hts_view(G)
    idb, idbB = load_const(P, [128, 128], BF16, G["identb"])
    wring = Ring(P, [128, 8, 8, 128], BF16, 2)
    prmring = Ring(P, [128, 16], F32, 2)
    htring = Ring(P, [128, 8, 512], BF16, 3)
    psring = Ring(P, [128, 512], F32, 6, psum=True)
    tpring = Ring(P, [128, 8, 128], BF16, 2, psum=True)
    pj = [P.sb([128, L + 2], BF16) for _ in range(6)]
    pjB = [Buf() for _ in range(6)]
    for k in range(6):
        P.op("pool", lambda e, k=k: e.memset(pj[k][:], 0.0), writes=[pjB[k]])
    gst, gstB = P.sb([128, 2, L], BF16), Buf()
    tmp, tmpB = P.sb([128, L], F32), Buf()
    tv, tvB = P.sb([128, L], F32), Buf()
    ya_st, yaB = P.sb([128, L], BF16), Buf()
    x0_st, x0B = P.sb([128, L], BF16), Buf()
    z_st, zB = P.sb([128, L], BF16), Buf()
    zt_st, ztB = P.sb([128, 32, 128], BF16), Buf()
    dOut = Buf(multi=True)
    boff = [0, 1024, 2048, 3072, 4096, 5120, 6144, 7168]
    def load_unit(cc):
        wb, wbB = wring.next()
        for blk in range(8):
            c0 = boff[blk] + cc * 128
            P.dma("pool", wb[:, :, blk, :], wv[:, :, c0:c0 + 128], writes=[wbB], key=wbB)
        prm, prmB = prmring.next()
        P.dma("sp", prm[:], G["cprm"][l, cc], writes=[prmB], key=prmB)
        return wb, wbB, prm, prmB
    nxt = load_unit(0)
    for b in range(NBC):
        for cc in range(8):
            wb, wbB, prm, prmB = nxt
            if not (b == NBC - 1 and cc == 7):
                nxt = load_unit((cc + 1) % 8)
            for tt in range(8):
                ht, htB = htring.next()
                P.dma("sp", ht[:], HTv[:, :, b * L + tt * 512: b * L + (tt + 1) * 512], writes=[htB], key=htB)
                for blk in range(8):
                    ps, psB = psring.next()

                    def mm(e, ps=ps, wb=wb, ht=ht, blk=blk):
                        for kc in range(8):
                            ins = e.matmul(ps[:], wb[:, kc, blk, :], ht[:, kc, :], start=(kc == 0), stop=(kc == 7))
                        return ins
                    P.op("pe", mm, reads=[wbB, htB], writes=[psB])
                    if blk < 6:
                        P.op("act", lambda e, ps=ps, blk=blk, tt=tt: e.copy(pj[blk][:, 1 + tt * 512: 1 + (tt + 1) * 512], ps[:]),
                             reads=[psB], writes=[pjB[blk]])
                    else:
                        P.op("act", lambda e, ps=ps, blk=blk, tt=tt: e.activation(
                            gst[:, blk - 6, tt * 512:(tt + 1) * 512], ps[:], AF.Sigmoid), reads=[psB], writes=[gstB])
            c1 = slice(1, L + 1)
            cm = slice(0, L)
            cp = slice(2, L + 2)
            P.op("pool", lambda e: e.tensor_tensor(out=pj[1][:, c1], in0=pj[1][:, c1], in1=pj[2][:, c1], op=ALU.mult),
                 reads=[pjB[1], pjB[2]], writes=[pjB[1]])
            P.op("pool", lambda e, prm=prm: e.tensor_scalar(tmp[:], pj[1][:, cm], prm[:, 0:1], 0.0, op0=ALU.mult, op1=ALU.add),
                 reads=[pjB[1], prmB], writes=[tmpB])
            P.op("dve", lambda e, prm=prm: e.scalar_tensor_tensor(out=tmp[:], in0=pj[1][:, c1], scalar=prm[:, 1:2], in1=tmp[:],
                                                                  op0=ALU.mult, op1=ALU.add),
                 reads=[pjB[1], prmB, tmpB], writes=[tmpB])
            P.op("dve", lambda e, prm=prm: e.scalar_tensor_tensor(out=tmp[:], in0=pj[1][:, cp], scalar=prm[:, 2:3], in1=tmp[:],
                                                                  op0=ALU.mult, op1=ALU.add),
                 reads=[pjB[1], prmB, tmpB], writes=[tmpB])
            P.op("pool", lambda e: e.tensor_tensor(out=ya_st[:], in0=pj[0][:, c1], in1=tmp[:], op=ALU.mult),
                 reads=[pjB[0], tmpB], writes=[yaB])
            P.dma("pool", G["YA"][cc * 128:(cc + 1) * 128, b * L:(b + 1) * L], ya_st[:], reads=[yaB], writes=[dOut], key=yaB)

            def conv_h(src, srcB, k, dst, dstB):
                P.op("pool", lambda e, prm=prm: e.tensor_scalar(tmp[:] if dst is None else dst[:], src[:, cm],
                                                               prm[:, 3 + 3 * k:4 + 3 * k], prm[:, 12 + k:13 + k],
                                                               op0=ALU.mult, op1=ALU.add),
                     reads=[srcB, prmB], writes=[dstB])
            conv_h(pj[3], pjB[3], 0, tv, tvB)
            for j, sl in ((1, c1), (2, cp)):
                P.op("dve", lambda e, prm=prm, j=j, sl=sl: e.scalar_tensor_tensor(
                    out=tv[:], in0=pj[3][:, sl], scalar=prm[:, 3 + j:4 + j], in1=tv[:], op0=ALU.mult, op1=ALU.add),
                    reads=[pjB[3], prmB, tvB], writes=[tvB])
            conv_h(pj[4], pjB[4], 1, tmp, tmpB)
            for j, sl in ((1, c1), (2, cp)):
                P.op("dve", lambda e, prm=prm, j=j, sl=sl: e.scalar_tensor_tensor(
                    out=tmp[:], in0=pj[4][:, sl], scalar=prm[:, 6 + j:7 + j], in1=tmp[:], op0=ALU.mult, op1=ALU.add),
                    reads=[pjB[4], prmB, tmpB], writes=[tmpB])
            P.op("pool", lambda e: e.tensor_tensor(out=z_st[:], in0=tv[:], in1=tmp[:], op=ALU.mult),
                 reads=[tvB, tmpB], writes=[zB])
            conv_h(pj[5], pjB[5], 2, tmp, tmpB)
            P.op("dve", lambda e, prm=prm: e.scalar_tensor_tensor(
                out=tmp[:], in0=pj[5][:, c1], scalar=prm[:, 10:11], in1=tmp[:], op0=ALU.mult, op1=ALU.add),
                reads=[pjB[5], prmB, tmpB], writes=[tmpB])
            P.op("dve", lambda e, prm=prm: e.scalar_tensor_tensor(
                out=x0_st[:], in0=pj[5][:, cp], scalar=prm[:, 11:12], in1=tmp[:], op0=ALU.mult, op1=ALU.add),
                reads=[pjB[5], prmB, tmpB], writes=[x0B])
            P.dma("pool", G["X0T"][cc * 128:(cc + 1) * 128, b * L:(b + 1) * L], x0_st[:], reads=[x0B], writes=[dOut], key=x0B)
            for q in range(4):
                tp, tpB = tpring.next()

                def ft(e, tp=tp, q=q):
                    for j in range(8):
                        sc = q * 8 + j
                        ins = e.transpose(tp[:, j, :], z_st[:, sc * 128:(sc + 1) * 128], idb[:])
                    return ins
                P.op("pe", ft, reads=[zB, idbB], writes=[tpB])
                P.op("act", lambda e, tp=tp, q=q: e.copy(zt_st[:, q * 8:(q + 1) * 8, :], tp[:, :, :]), reads=[tpB], writes=[ztB])
            P.dma("pool", G["Z"][b, cc], zt_st[:].rearrange("p a c -> p (a c)"), reads=[ztB], writes=[dOut], key=ztB)
            for k in range(2):
                P.dma("pool", G["GT"][k * D + cc * 128: k * D + (cc + 1) * 128, b * L:(b + 1) * L], gst[:, k, :],
                      reads=[gstB], writes=[dOut], key=gstB)
    P.finish()


def phase_f(nc, G, l):
    P = Phase(nc, f"f{l}")
    posz, poszB = load_const(P, [33, L], F32, G["posz"])
    w1, w1B = load_const(P, [33, 64], F32, G["flt_w1"][l])
    w2, w2B = load_const(P, [64, 64], F32, G["flt_w2"][l])
    w3, w3B = load_const(P, [64, 2048], F32, G["flt_w3"][l])
    fp, fpB = load_const(P, [64, 4], F32, G["fprm"][l])
    tcol, tcolB = load_const(P, [128, 32], F32, G["tcol"])
    ndl, ndlB = load_const(P, [128, D], F32, G["negdelta"].partition_broadcast(128))
    hb_, hbB_ = load_const(P, [128, D], F32, G["hyena_bias"][l:l + 1, :].partition_broadcast(128))
    ones, onesB = load_const(P, [128, 128], F32, G["ones"])
    nyq, nyqB = load_const(P, [128, 32], BF16, G["nyqF"])
    eps6, eps6B = load_const(P, [128, 1], F32, G["eps6"])
    h1, h1B = P.sb([64, L], F32), Buf()
    h2, h2B = P.sb([64, L], F32), Buf()
    wr, wrB = P.sb([64, L], F32), Buf()
    psr = Ring(P, [128, 512], F32, 4, psum=True)
    ssq = [P.ps([128, 512], F32) for _ in range(4)]
    ssqB = [Buf() for _ in range(4)]
    dKR = Buf(multi=True)

    def sin_layer(src, srcB, w, wB, K, col, dst, dstB):
        for tt in range(8):
            ps, psB = psr.next()
            P.op("pe", lambda e, ps=ps, tt=tt: e.matmul(ps[0:64, :], w[0:K, :], src[0:K, tt * 512:(tt + 1) * 512],
                                                        start=True, stop=True), reads=[srcB, wB], writes=[psB])
            sl = slice(tt * 512, (tt + 1) * 512)
            P.op("dve", lambda e, ps=ps, sl=sl: e.tensor_scalar(dst[:, sl], ps[0:64, :], fp[:, col:col + 1], fp[:, col + 1:col + 2],
                                                                op0=ALU.add, op1=ALU.mult), reads=[psB, fpB], writes=[dstB])
        for _ in range(2):
            P.op("dve", lambda e: e.tensor_scalar(wr[:], dst[:], PI, -2 * PI, op0=ALU.is_gt, op1=ALU.mult), reads=[dstB], writes=[wrB])
            P.op("dve", lambda e: e.tensor_tensor(out=dst[:], in0=dst[:], in1=wr[:], op=ALU.add), reads=[dstB, wrB], writes=[dstB])
            P.op("dve", lambda e: e.tensor_scalar(wr[:], dst[:], -PI, 2 * PI, op0=ALU.is_lt, op1=ALU.mult), reads=[dstB], writes=[wrB])
            P.op("dve", lambda e: e.tensor_tensor(out=dst[:], in0=dst[:], in1=wr[:], op=ALU.add), reads=[dstB, wrB], writes=[dstB])
        P.op("dve", lambda e: e.tensor_scalar(dst[:], dst[:], 3.141592, -3.141592, op0=ALU.min, op1=ALU.max),
             reads=[dstB], writes=[dstB])
        P.op("act", lambda e: e.activation(dst[:], dst[:], AF.Sin), reads=[dstB], writes=[dstB])

    sin_layer(posz, poszB, w1, w1B, 33, 0, h1, h1B)
    sin_layer(h1, h1B, w2, w2B, 64, 2, h2, h2B)

    decr = Ring(P, [128, D], F32, 1)
    krr = Ring(P, [128, 512], F32, 2)
    sqr = Ring(P, [128, 512], F32, 2)
    for sc in range(32):
        dec, decB = decr.next()
        P.op("act", lambda e, dec=dec, sc=sc: e.activation(dec[:], ndl[:], AF.Exp, scale=tcol[:, sc:sc + 1]),
             reads=[ndlB, tcolB], writes=[decB])
        for q in range(4):
            ps, psB = psr.next()
            P.op("pe", lambda e, ps=ps, sc=sc, q=q: e.matmul(ps[:], h2[:, sc * 128:(sc + 1) * 128], w3[:, q * 512:(q + 1) * 512],
                                                             start=True, stop=True), reads=[h2B, w3B], writes=[psB])
            kr, krB = krr.next()
            ch0 = (q % 2) * 512
            P.op("dve", lambda e, ps=ps, kr=kr, dec=dec, ch0=ch0: e.tensor_tensor(out=kr[:], in0=ps[:], in1=dec[:, ch0:ch0 + 512],
                                                                                  op=ALU.mult), reads=[psB, decB], writes=[krB])
            P.dma("pool", G["KRAW"][sc * 128:(sc + 1) * 128, q * 512:(q + 1) * 512], kr[:], reads=[krB], writes=[dKR], key=krB)
            sq, sqB = sqr.next()
            P.op("act", lambda e, sq=sq, kr=kr: e.activation(sq[:], kr[:], AF.Square), reads=[krB], writes=[sqB])
            P.op("pe", lambda e, sq=sq, q=q, sc=sc: e.matmul(ssq[q][:], ones[:], sq[:], start=(sc == 0), stop=(sc == 31)),
                 reads=[sqB, onesB], writes=[ssqB[q]])
    rn, rnB = P.sb([128, 4, 512], F32), Buf()
    for q in range(4):
        P.op("act", lambda e, q=q: e.activation(rn[:, q, :], ssq[q][:], AF.Sqrt, bias=eps6[:, 0:1], scale=1.0),
             reads=[ssqB[q], eps6B], writes=[rnB])
    P.op("dve", lambda e: e.reciprocal(rn[:], rn[:]), reads=[rnB], writes=[rnB])

    A, AB = P.sb([128, 32, 512], BF16), Buf()
    Bm, BmB = P.sb([128, 32, 512], BF16), Buf()
    ldr = Ring(P, [128, 2, 512], F32, 2)
    fring = Ring(P, [128, 32, 128], BF16, 2)
    kor = Ring(P, [128, 512], F32, 2)
    dK = Buf(multi=True)
    for hh in range(2):
        for sc in range(32):
            ld, ldB = ldr.next()
            for d in range(2):
                q = d * 2 + hh
                P.dma("sp", ld[:, d, :], G["KRAW"][sc * 128:(sc + 1) * 128, q * 512:(q + 1) * 512], reads=[dKR], writes=[ldB], key=ldB)
            P.op("dve", lambda e, ld=ld, hh=hh: e.tensor_tensor(out=ld[:, 0, :], in0=ld[:, 0, :], in1=rn[:, hh, :], op=ALU.mult),
                 reads=[ldB, rnB], writes=[ldB])
            P.op("dve", lambda e, ld=ld, hh=hh: e.tensor_tensor(out=ld[:, 1, :], in0=ld[:, 1, :], in1=rn[:, 2 + hh, :], op=ALU.mult),
                 reads=[ldB, rnB], writes=[ldB])
            if sc == 0:
                P.op("dve", lambda e, ld=ld: e.memset(ld[0:1, 1, :], 0.0), reads=[ldB], writes=[ldB])
            P.op("pool", lambda e, ld=ld, sc=sc: e.tensor_tensor(out=A[:, sc, :], in0=ld[:, 0, :], in1=ld[:, 1, :], op=ALU.add),
                 reads=[ldB], writes=[AB])
            P.op("pool", lambda e, ld=ld, sc=sc: e.tensor_tensor(out=Bm[:, sc, :], in0=ld[:, 0, :], in1=ld[:, 1, :], op=ALU.subtract),
                 reads=[ldB], writes=[BmB])
        for fc in range(32):
            for kind in range(2):
                fb, fbB = fring.next()
                P.dma("sp", fb[:], G["dftF"][kind * 32 + fc], writes=[fbB], key=fbB)
                ps, psB = psr.next()
                src, srcB = (A, AB) if kind == 0 else (Bm, BmB)

                def mm(e, ps=ps, fb=fb, src=src):
                    for sc in range(32):
                        ins = e.matmul(ps[:], fb[:, sc, :], src[:, sc, :], start=(sc == 0), stop=(sc == 31))
                    return ins
                P.op("pe", mm, reads=[fbB, srcB], writes=[psB])
                ko, koB = kor.next()
                if kind == 0:
                    P.op("dve", lambda e, ko=ko, ps=ps, hh=hh: e.tensor_tensor(out=ko[:], in0=ps[:], in1=hb_[:, hh * 512:(hh + 1) * 512],
                                                                               op=ALU.add), reads=[psB, hbB_], writes=[koB])
                else:
                    P.op("act", lambda e, ko=ko, ps=ps: e.copy(ko[:], ps[:]), reads=[psB], writes=[koB])
                dst = G["KC"] if kind == 0 else G["KS"]
                P.dma("pool", dst[fc * 128:(fc + 1) * 128, hh * 512:(hh + 1) * 512], ko[:], reads=[koB], writes=[dK], key=koB)
        ps, psB = psr.next()

        def mmn(e, ps=ps):
            for sc in range(32):
                ins = e.matmul(ps[0:1, :], nyq[:, sc:sc + 1], A[:, sc, :], start=(sc == 0), stop=(sc == 31))
            return ins
        P.op("pe", mmn, reads=[nyqB, AB], writes=[psB])
        ko, koB = kor.next()
        P.op("dve", lambda e, ko=ko, ps=ps, hh=hh: e.tensor_tensor(out=ko[0:1, :], in0=ps[0:1, :], in1=hb_[0:1, hh * 512:(hh + 1) * 512],
                                                                   op=ALU.add), reads=[psB, hbB_], writes=[koB])
        P.dma("pool", G["KN"][0:1, hh * 512:(hh + 1) * 512], ko[0:1, :], reads=[koB], writes=[dK], key=koB)
    P.finish()


def phase_h(nc, G, l):
    P = Phase(nc, f"h{l}")
    nyqF, nyqFB = load_const(P, [128, 32], BF16, G["nyqF"])
    nyqG, nyqGB = load_const(P, [1, L], BF16, G["nyqG"])
    z_sb, zB = P.sb([128, 4, 32, 128], BF16), Buf()
    fring = Ring(P, [128, 32, 128], BF16, 3)
    kring = Ring(P, [128, 2, 512], F32, 2)
    tring = Ring(P, [128, 512], F32, 8)
    Y, YB = P.sb([128, 2, 32, 512], BF16), Buf()
    Yn, YnB = P.sb([1, 512], BF16), Buf()
    knt, kntB = P.sb([1, 512], F32), Buf()
    gring = Ring(P, [128, 16, 512], BF16, 2)
    x0r = Ring(P, [128, 512], BF16, 3)
    outr = Ring(P, [128, 512], BF16, 3)
    psz = Ring(P, [128, 512], F32, 4, psum=True)
    acc = [P.ps([128, 512], F32) for _ in range(4)]
    accB = [Buf() for _ in range(4)]
    dOut = Buf(multi=True)
    for b in range(NBC):
        for g in range(2):
            for j in range(4):
                P.dma("sp", z_sb[:, j, :, :].rearrange("p a c -> p (a c)"), G["Z"][b, g * 4 + j], writes=[zB], key=zB)
            P.dma("sp", knt[:], G["KN"][0:1, g * 512:(g + 1) * 512], writes=[kntB], key=kntB)
            for fc in range(32):
                kt, ktB = kring.next()
                P.dma("sp", kt[:, 0, :], G["KC"][fc * 128:(fc + 1) * 128, g * 512:(g + 1) * 512], writes=[ktB], key=ktB)
                P.dma("sp", kt[:, 1, :], G["KS"][fc * 128:(fc + 1) * 128, g * 512:(g + 1) * 512], writes=[ktB], key=ktB)
                zp = []
                for kind in range(2):
                    fb, fbB = fring.next()
                    P.dma("sp", fb[:], G["dftF"][kind * 32 + fc], writes=[fbB], key=fbB)
                    ps, psB = psz.next()

                    def mm(e, ps=ps, fb=fb):
                        for sc in range(32):
                            ins = e.matmul(ps[:], fb[:, sc, :], z_sb[:, :, sc, :], start=(sc == 0), stop=(sc == 31))
                        return ins
                    P.op("pe", mm, reads=[fbB, zB], writes=[psB])
                    zp.append((ps, psB))
                (zc, zcB), (zs, zsB) = zp
                t = [tring.next() for _ in range(4)]
                P.op("dve", lambda e, zc=zc, kt=kt, t=t: e.tensor_tensor(out=t[0][0][:], in0=zc[:], in1=kt[:, 0, :], op=ALU.mult),
                     reads=[zcB, ktB], writes=[t[0][1]])
                P.op("dve", lambda e, zs=zs, kt=kt, t=t: e.tensor_tensor(out=t[1][0][:], in0=zs[:], in1=kt[:, 1, :], op=ALU.mult),
                     reads=[zsB, ktB], writes=[t[1][1]])
                P.op("dve", lambda e, zc=zc, kt=kt, t=t: e.tensor_tensor(out=t[2][0][:], in0=zc[:], in1=kt[:, 1, :], op=ALU.mult),
                     reads=[zcB, ktB], writes=[t[2][1]])
                P.op("dve", lambda e, zs=zs, kt=kt, t=t: e.tensor_tensor(out=t[3][0][:], in0=zs[:], in1=kt[:, 0, :], op=ALU.mult),
                     reads=[zsB, ktB], writes=[t[3][1]])
                P.op("pool", lambda e, t=t, fc=fc: e.tensor_tensor(out=Y[:, 0, fc, :], in0=t[0][0][:], in1=t[1][0][:], op=ALU.subtract),
                     reads=[t[0][1], t[1][1]], writes=[YB])
                P.op("pool", lambda e, t=t, fc=fc: e.tensor_tensor(out=Y[:, 1, fc, :], in0=t[2][0][:], in1=t[3][0][:], op=ALU.add),
                     reads=[t[2][1], t[3][1]], writes=[YB])
            ps, psB = psz.next()

            def mmn(e, ps=ps):
                for sc in range(32):
                    ins = e.matmul(ps[0:1, :], nyqF[:, sc:sc + 1], z_sb[:, :, sc, :], start=(sc == 0), stop=(sc == 31))
                return ins
            P.op("pe", mmn, reads=[nyqFB, zB], writes=[psB])
            P.op("dve", lambda e, ps=ps: e.tensor_tensor(out=Yn[0:1, :], in0=ps[0:1, :], in1=knt[0:1, :], op=ALU.mult),
                 reads=[psB, kntB], writes=[YnB])
            for tt in range(8):
                for piece in range(4):
                    gp, gpB = gring.next()
                    P.dma("sp", gp[:], G["dftG"][tt, piece], writes=[gpB], key=gpB)
                    kind = piece // 2
                    for j in range(4):
                        def mmi(e, gp=gp, j=j, piece=piece, kind=kind, tt=tt):
                            for i in range(16):
                                fc = (piece % 2) * 16 + i
                                ins = e.matmul(acc[j][:], Y[:, kind, fc, j * 128:(j + 1) * 128], gp[:, i, :],
                                               start=(piece == 0 and i == 0), stop=False)
                            if piece == 3:
                                ins = e.matmul(acc[j][:], Yn[0:1, j * 128:(j + 1) * 128], nyqG[0:1, tt * 512:(tt + 1) * 512],
                                               start=False, stop=True)
                            return ins
                        rd = [gpB, YB] + ([YnB, nyqGB] if piece == 3 else [])
                        P.op("pe", mmi, reads=rd, writes=[accB[j]])
                for j in range(4):
                    ch0 = (g * 4 + j) * 128
                    x0t, x0B = x0r.next()
                    P.dma("sp", x0t[:], G["X0T"][ch0:ch0 + 128, b * L + tt * 512: b * L + (tt + 1) * 512], writes=[x0B], key=x0B)
                    ot, otB = outr.next()
                    P.op("dve", lambda e, ot=ot, j=j, x0t=x0t: e.tensor_tensor(out=ot[:], in0=acc[j][:], in1=x0t[:], op=ALU.mult),
                         reads=[accB[j], x0B], writes=[otB])
                    P.dma("pool", G["YH"][ch0:ch0 + 128, b * L + tt * 512: b * L + (tt + 1) * 512], ot[:], reads=[otB],
                          writes=[dOut], key=otB)
    P.finish()


def load_w_cast(P, dst, dstB, src_ap, nk, ncols, col0=0):
    v = src_ap.rearrange("(kc p) n -> p kc n", p=128)
    for kc in range(nk):
        for c in range(0, ncols, 2048):
            w = min(2048, ncols - c)
            P.dma("pool", dst[:, kc, c:c + w], v[:, kc, col0 + c:col0 + c + w], writes=[dstB], key=dstB)


def phase_o(nc, G, l, want_logits):
    P = Phase(nc, f"o{l}")
    C = LNCtx(P, G, G["ln_mix_g"][l:l + 1, :], G["ln_mix_b"][l:l + 1, :])
    wa, waB = P.sb([128, 8, D], BF16), Buf()
    wh, whB = P.sb([128, 8, D], BF16), Buf()
    wo, woB = P.sb([128, 8, D], BF16), Buf()
    load_w_cast(P, wa, waB, G["w_a_out"][l], 8, D)
    load_w_cast(P, wh, whB, G["w_h_out"][l], 8, D)
    load_w_cast(P, wo, woB, G["w_o"][l], 8, D)
    yar = Ring(P, [128, 8, 512], BF16, 2)
    yhr = Ring(P, [128, 8, 512], BF16, 2)
    gtr = Ring(P, [128, 16, 512], BF16, 2)
    mgr = Ring(P, [128, 8, 512], BF16, 2)
    t1r = Ring(P, [128, 512], F32, 2)
    t2r = Ring(P, [128, 512], F32, 2)
    hin = Ring(P, [128, D], F32, 3)
    rr = Ring(P, [128, D], F32, 2)
    hnr = Ring(P, [128, D], F32, 2)
    psp = Ring(P, [128, 512], F32, 4, psum=True)
    pso = Ring(P, [128, 512], F32, 2, psum=True)
    YAv = G["YA"].rearrange("(kc p) t -> p kc t", p=128)
    YHv = G["YH"].rearrange("(kc p) t -> p kc t", p=128)
    GTv = G["GT"].rearrange("(kc p) t -> p kc t", p=128)
    HTv = ht_view(G)
    dH, dHT = Buf(multi=True), Buf(multi=True)
    if want_logits:
        idf, idfB = load_const(P, [128, 128], F32, G["identf"])
        rt, rtB = load_const(P, [128, 8, NE], F32, G["moe_router"][0].rearrange("(kc p) e -> p kc e", p=128))
        hTf = Ring(P, [128, 8, 128], F32, 2)
        lgr = Ring(P, [128, NE], F32, 2)
        dLG = Buf(multi=True)
    for tt in range(T // 512):
        ts = slice(tt * 512, (tt + 1) * 512)
        ya, yaB = yar.next()
        yh, yhB = yhr.next()
        gt, gtB = gtr.next()
        P.dma("sp", ya[:], YAv[:, :, ts], writes=[yaB], key=yaB)
        P.dma("sp", yh[:], YHv[:, :, ts], writes=[yhB], key=yhB)
        P.dma("sp", gt[:], GTv[:, :, ts], writes=[gtB], key=gtB)
        mg, mgB = mgr.next()
        for j in range(8):
            pa, paB = psp.next()
            ph, phB = psp.next()

            def mma(e, pa=pa, ya=ya, j=j):
                for kc in range(8):
                    ins = e.matmul(pa[:], wa[:, kc, j * 128:(j + 1) * 128], ya[:, kc, :], start=(kc == 0), stop=(kc == 7))
                return ins

            def mmh(e, ph=ph, yh=yh, j=j):
                for kc in range(8):
                    ins = e.matmul(ph[:], wh[:, kc, j * 128:(j + 1) * 128], yh[:, kc, :], start=(kc == 0), stop=(kc == 7))
                return ins
            P.op("pe", mma, reads=[waB, yaB], writes=[paB])
            P.op("pe", mmh, reads=[whB, yhB], writes=[phB])
            t1, t1B = t1r.next()
            t2, t2B = t2r.next()
            P.op("dve", lambda e, t1=t1, pa=pa, gt=gt, j=j: e.tensor_tensor(out=t1[:], in0=pa[:], in1=gt[:, j, :], op=ALU.mult),
                 reads=[paB, gtB], writes=[t1B])
            P.op("dve", lambda e, t2=t2, ph=ph, gt=gt, j=j: e.tensor_tensor(out=t2[:], in0=ph[:], in1=gt[:, 8 + j, :], op=ALU.mult),
                 reads=[phB, gtB], writes=[t2B])
            P.op("pool", lambda e, t1=t1, t2=t2, mg=mg, j=j: e.tensor_tensor(out=mg[:, j, :], in0=t1[:], in1=t2[:], op=ALU.add),
                 reads=[t1B, t2B], writes=[mgB])
        stage, stageB = C.stage.next()
        for i in range(4):
            t0 = tt * 512 + i * 128
            hi, hiB = hin.next()
            P.dma("sp", hi[:], G["Hsrc"][t0:t0 + 128, :], writes=[hiB], key=hiB)
            r, rB = rr.next()
            for hf in range(2):
                po, poB = pso.next()

                def mmo(e, po=po, mg=mg, i=i, hf=hf):
                    for kc in range(8):
                        ins = e.matmul(po[:], mg[:, kc, i * 128:(i + 1) * 128], wo[:, kc, hf * 512:(hf + 1) * 512],
                                       start=(kc == 0), stop=(kc == 7))
                    return ins
                P.op("pe", mmo, reads=[mgB, woB], writes=[poB])
                P.op("dve", lambda e, r=r, hi=hi, po=po, hf=hf: e.scalar_tensor_tensor(
                    out=r[:, hf * 512:(hf + 1) * 512], in0=hi[:, hf * 512:(hf + 1) * 512], scalar=float(ALPHA), in1=po[:],
                    op0=ALU.mult, op1=ALU.add), reads=[hiB, poB], writes=[rB])
            hn, hnB = hnr.next()
            C.norm(r, rB, hn, hnB)
            P.dma("pool", G["H"][t0:t0 + 128, :], hn[:], reads=[hnB], writes=[dH], key=hnB)
            C.transpose_into(hn, hnB, stage, stageB, i)
            if want_logits:
                hT, hTB = hTf.next()
                for q in range(2):
                    pt, ptB = pso.next()

                    def ftf(e, pt=pt, hn=hn, q=q):
                        for jj in range(4):
                            j = q * 4 + jj
                            ins = e.transpose(pt[:, jj * 128:(jj + 1) * 128], hn[:, j * 128:(j + 1) * 128], idf[:])
                        return ins
                    P.op("pe", ftf, reads=[hnB, idfB], writes=[ptB])
                    P.op("act", lambda e, pt=pt, hT=hT, q=q: e.copy(hT[:, q * 4:(q + 1) * 4, :],
                                                                   pt[:].rearrange("p (a c) -> p a c", c=128)),
                         reads=[ptB], writes=[hTB])
                pl, plB = pso.next()

                def mml(e, pl=pl, hT=hT):
                    for kc in range(8):
                        ins = e.matmul(pl[:, 0:NE], hT[:, kc, :], rt[:, kc, :], start=(kc == 0), stop=(kc == 7))
                    return ins
                P.op("pe", mml, reads=[hTB, rtB], writes=[plB])
                lg, lgB = lgr.next()
                P.op("act", lambda e, lg=lg, pl=pl: e.copy(lg[:], pl[:, 0:NE]), reads=[plB], writes=[lgB])
                P.dma("pool", G["LG"][t0:t0 + 128, :], lg[:], reads=[lgB], writes=[dLG], key=lgB)
        P.dma("pool", HTv[:, :, ts], stage[:], reads=[stageB], writes=[dHT], key=stageB)
    P.finish()


def phase_ffn(nc, G, l, moe):
    P = Phase(nc, f"ffn{l}")
    TT = 512
    nff = (DFE if moe else DFF) // 128
    C = LNCtx(P, G, G["ln_ffn_g"][l:l + 1, :], G["ln_ffn_b"][l:l + 1, :], want_T=not moe)
    HTv = ht_view(G)
    HTs = hts_view(G)
    htr = Ring(P, [128, 8, TT], BF16, 2)
    aT, aTB = P.sb([128, nff, TT], BF16), Buf()
    w13r = Ring(P, [128, 2, 8, 128], BF16, 3)
    w2c = Ring(P, [128, 512], BF16, 4)
    sg = Ring(P, [128, TT], F32, 2)
    psg = Ring(P, [128, 512], F32, 4, psum=True)
    hin = Ring(P, [128, D], F32, 3)
    accr = Ring(P, [128, D], F32, 5)
    hnr = Ring(P, [128, D], F32, 2)
    dH, dHT = Buf(multi=True), Buf(multi=True)
    if moe:
        lgr = Ring(P, [128, NE], F32, 5)
        cwr = Ring(P, [128, NE], F32, 5)
        m8r = Ring(P, [128, 8], F32, 2)
        scr = Ring(P, [128, 3 * NE], F32, 2)
    nexp = NE if moe else 1
    for tt in range(T // TT):
        ts = slice(tt * TT, (tt + 1) * TT)
        ht, htB = htr.next()
        P.dma("sp", ht[:], HTs[:, :, ts], writes=[htB], key=htB)
        accs = []
        cws = []
        for i in range(TT // 128):
            t0 = tt * TT + i * 128
            hi, hiB = hin.next()
            P.dma("sp", hi[:], G["Hsrc"][t0:t0 + 128, :], writes=[hiB], key=hiB)
            ac, acB = accr.next()
            P.op("pool", lambda e, ac=ac, hi=hi: e.tensor_scalar(ac[:], hi[:], float(ALPHA), 0.0, op0=ALU.mult, op1=ALU.add),
                 reads=[hiB], writes=[acB])
            accs.append((ac, acB))
            if moe:
                lg, lgB = lgr.next()
                P.dma("sp", lg[:], G["LGsrc"][t0:t0 + 128, :], writes=[lgB], key=lgB)
                cw, cwB = cwr.next()
                m8, m8B = m8r.next()
                sc_, scB = scr.next()
                P.op("dve", lambda e, m8=m8, lg=lg: e.max(m8[:], lg[:]), reads=[lgB], writes=[m8B])
                P.op("dve", lambda e, sc_=sc_, lg=lg, m8=m8: e.tensor_scalar(sc_[:, 0:NE], lg[:], m8[:, 1:2], None, op0=ALU.is_ge),
                     reads=[lgB, m8B], writes=[scB])
                P.op("dve", lambda e, sc_=sc_, m8=m8: e.tensor_scalar(sc_[:, 2 * NE:2 * NE + 1], m8[:, 0:1], -1.0, None, op0=ALU.mult),
                     reads=[m8B, scB], writes=[scB])
                P.op("act", lambda e, sc_=sc_, lg=lg: e.activation(sc_[:, NE:2 * NE], lg[:], AF.Exp, bias=sc_[:, 2 * NE:2 * NE + 1], scale=1.0),
                     reads=[lgB, scB], writes=[scB])
                P.op("dve", lambda e, sc_=sc_: e.tensor_tensor(out=sc_[:, NE:2 * NE], in0=sc_[:, NE:2 * NE], in1=sc_[:, 0:NE], op=ALU.mult),
                     reads=[scB], writes=[scB])
                P.op("dve", lambda e, sc_=sc_: e.tensor_reduce(out=sc_[:, 2 * NE + 1:2 * NE + 2], in_=sc_[:, NE:2 * NE],
                                                               axis=mybir.AxisListType.X, op=ALU.add), reads=[scB], writes=[scB])
                P.op("dve", lambda e, sc_=sc_: e.reciprocal(sc_[:, 2 * NE + 1:2 * NE + 2], sc_[:, 2 * NE + 1:2 * NE + 2]),
                     reads=[scB], writes=[scB])
                P.op("dve", lambda e, sc_=sc_, cw=cw: e.tensor_scalar(cw[:], sc_[:, NE:2 * NE], sc_[:, 2 * NE + 1:2 * NE + 2], None, op0=ALU.mult),
                     reads=[scB], writes=[cwB])
                cws.append((cw, cwB))
        for ex in range(nexp):
            if moe:
                W1, W3, W2 = G["moe_w1"][0, ex], G["moe_w3"][0, ex], G["moe_w2"][0, ex]
            else:
                W1, W3, W2 = G["ffn_w1"][0], G["ffn_w3"][0], G["ffn_w2"][0]
            w1v = W1.rearrange("(kc p) n -> p kc n", p=128)
            w3v = W3.rearrange("(kc p) n -> p kc n", p=128)
            for c in range(nff):
                w13, w13B = w13r.next()
                P.dma("pool", w13[:, 0, :, :], w1v[:, :, c * 128:(c + 1) * 128], writes=[w13B], key=w13B)
                P.dma("pool", w13[:, 1, :, :], w3v[:, :, c * 128:(c + 1) * 128], writes=[w13B], key=w13B)
                pg, pgB = psg.next()
                pu, puB = psg.next()

                def mmg(e, pg=pg, w13=w13, ht=ht, which=0):
                    for kc in range(8):
                        ins = e.matmul(pg[:], w13[:, which, kc, :], ht[:, kc, :], start=(kc == 0), stop=(kc == 7))
                    return ins
                P.op("pe", mmg, reads=[w13B, htB], writes=[pgB])
                P.op("pe", lambda e, pu=pu, w13=w13, ht=ht: mmg(e, pu, w13, ht, 1), reads=[w13B, htB], writes=[puB])
                s_, sB = sg.next()
                P.op("act", lambda e, s_=s_, pg=pg: e.activation(s_[:], pg[:], AF.Silu), reads=[pgB], writes=[sB])
                P.op("dve", lambda e, s_=s_, pu=pu, c=c: e.tensor_tensor(out=aT[:, c, :], in0=pu[:], in1=s_[:], op=ALU.mult),
                     reads=[puB, sB], writes=[aTB])
            for hf in range(2):
                pyl = [psg.next() for _ in range(TT // 128)]
                for c in range(nff):
                    w2t, w2B = w2c.next()
                    P.dma("pool", w2t[:, 0:512], W2[c * 128:(c + 1) * 128, hf * 512:(hf + 1) * 512], writes=[w2B], key=w2B)
                    for i in range(TT // 128):
                        py, pyB = pyl[i]
                        P.op("pe", lambda e, py=py, i=i, c=c, w2t=w2t: e.matmul(py[:], aT[:, c, i * 128:(i + 1) * 128], w2t[:, 0:512],
                                                                               start=(c == 0), stop=(c == nff - 1)),
                             reads=[aTB, w2B], writes=[pyB])
                for i in range(TT // 128):
                    py, pyB = pyl[i]
                    ac, acB = accs[i]
                    hs = slice(hf * 512, (hf + 1) * 512)
                    if moe:
                        cw, cwB = cws[i]
                        P.op("dve", lambda e, ac=ac, py=py, cw=cw, ex=ex, hs=hs: e.scalar_tensor_tensor(
                            out=ac[:, hs], in0=py[:], scalar=cw[:, ex:ex + 1], in1=ac[:, hs], op0=ALU.mult, op1=ALU.add),
                            reads=[pyB, cwB, acB], writes=[acB])
                    else:
                        P.op("dve", lambda e, ac=ac, py=py, hs=hs: e.tensor_tensor(out=ac[:, hs], in0=py[:], in1=ac[:, hs], op=ALU.add),
                             reads=[pyB, acB], writes=[acB])
        if not moe:
            stage, stageB = C.stage.next()
        for i in range(TT // 128):
            t0 = tt * TT + i * 128
            ac, acB = accs[i]
            hn, hnB = hnr.next()
            C.norm(ac, acB, hn, hnB)
            if moe:
                P.dma("act", G["out"][t0:t0 + 128, :], hn[:], reads=[hnB], writes=[dH], key=hnB)
            else:
                P.dma("act", G["H"][t0:t0 + 128, :], hn[:], reads=[hnB], writes=[dH], key=hnB)
                C.transpose_into(hn, hnB, stage, stageB, i)
        if not moe:
            P.dma("act", HTv[:, :, ts], stage[:], reads=[stageB], writes=[dHT], key=stageB)
    P.finish()


_CONST = {}


def host_consts():
    if _CONST:
        return _CONST
    bf = ml_dtypes.bfloat16
    s = np.arange(L, dtype=np.float64)
    f = np.arange(L, dtype=np.float64)
    ang = 2.0 * np.pi * np.outer(s, f) / NFFT
    Fm = np.concatenate([np.cos(ang), np.sin(ang)], axis=1)
    Fb = Fm.reshape(32, 128, 64, 128).transpose(2, 1, 0, 3)
    _CONST["dftF"] = np.ascontiguousarray(Fb).astype(bf)
    Gc = (2.0 / NFFT) * np.cos(ang.T)
    Gc[0, :] = 1.0 / NFFT
    Gs = (2.0 / NFFT) * np.sin(ang.T)
    Gm = np.concatenate([Gc, Gs], axis=0)
    Gb = Gm.reshape(4, 16, 128, 8, 512).transpose(3, 0, 2, 1, 4)
    _CONST["dftG"] = np.ascontiguousarray(Gb).astype(bf)
    sgn = np.where(np.arange(128) % 2 == 0, 1.0, -1.0)
    _CONST["nyqF"] = np.ascontiguousarray(np.repeat(sgn[:, None], 32, axis=1)).astype(bf)
    _CONST["nyqG"] = ((1.0 / NFFT) * np.where(np.arange(L) % 2 == 0, 1.0, -1.0))[None, :].astype(bf)
    t = np.linspace(0.0, 1.0, L, dtype=np.float32)[:, None]
    bands = np.linspace(1e-4, 16 - 1, 16, dtype=np.float32)[None, :]
    w = (np.float32(2.0 * math.pi / L) * np.arange(L, dtype=np.float32))[:, None]
    angp = bands * w
    z = np.concatenate([t, np.cos(angp), -np.sin(angp)], axis=-1).astype(np.float32)
    _CONST["posz"] = np.ascontiguousarray(z.T)
    _CONST["tcol"] = np.ascontiguousarray(t[:, 0].reshape(32, 128).T)
    max_decay = math.log(1e-2) / 0.3
    min_decay = math.log(1e-2) / 1.5
    deltas = np.linspace(min_decay, max_decay, D, dtype=np.float32)
    _CONST["negdelta"] = (-np.abs(deltas))[None, :].astype(np.float32)
    _CONST["identb"] = np.eye(128, dtype=np.float32).astype(bf)
    _CONST["identf"] = np.eye(128, dtype=np.float32)
    _CONST["ones"] = np.ones((128, 128), np.float32)
    _CONST["eps"] = np.full((128, 1), LN_EPS, np.float32)
    _CONST["eps6"] = np.full((128, 1), 1e-6, np.float32)
    return _CONST


WEIGHT_NAMES = ["ln_in_g", "ln_in_b", "w_in", "flt_w1", "flt_w2", "flt_w3", "hyena_bias", "w_a_out", "w_h_out", "w_o",
                "ln_mix_g", "ln_mix_b", "ffn_w1", "ffn_w3", "ffn_w2", "moe_router", "moe_w1", "moe_w3", "moe_w2",
                "ln_ffn_g", "ln_ffn_b"]

SCRATCH = {
    "H": ([T, D], F32), "HT": ([D, T], BF16), "YA": ([D, T], BF16), "X0T": ([D, T], BF16), "GT": ([2 * D, T], BF16),
    "Z": ([NBC, 8, 128, L], BF16), "KRAW": ([L, 2 * D], F32), "KC": ([L, D], F32), "KS": ([L, D], F32), "KN": ([1, D], F32),
    "YH": ([D, T], BF16), "LG": ([T, NE], F32),
}

PHASES = ["ln0", "a0", "f0", "h0", "o0", "ffn0", "a1", "f1", "h1", "o1", "ffn1"]


class LazyG(dict):
    def __init__(self, nc, shapes, outs):
        super().__init__()
        self.nc, self.shapes, self.outs = nc, shapes, outs
        self.used_inputs = []

    def __missing__(self, name):
        nc = self.nc
        if name in ("Hsrc", "HTsrc", "LGsrc"):
            base = name[:-3]
            ap = self[base + "in"] if (base + "in") in self.shapes else self[base]
        elif name in self.shapes:
            shape, dt = self.shapes[name]
            bdt = BF16 if dt == ml_dtypes.bfloat16 else F32
            ap = nc.dram_tensor(name, list(shape), bdt, kind="ExternalInput").ap()
            self.used_inputs.append(name)
        elif name == "out":
            ap = nc.dram_tensor("out", [T, D], F32, kind="ExternalOutput").ap()
        else:
            shape, dt = SCRATCH[name]
            kind = "ExternalOutput" if name in self.outs else "Internal"
            ap = nc.dram_tensor(name, list(shape), dt, kind=kind).ap()
        self[name] = ap
        return ap


def build(shapes, phases=None, dbg_out=()):
    nc = bass.Bass("TRN2", target_bir_lowering=False)
    G = LazyG(nc, shapes, set(dbg_out))
    phases = PHASES if phases is None else phases
    for ph in phases:
        if ph == "ln0":
            phase_ln0(nc, G)
        elif ph[0] == "a":
            phase_a(nc, G, int(ph[1]))
        elif ph[0] == "f" and ph[1] != "f":
            phase_f(nc, G, int(ph[1]))
        elif ph[0] == "h":
            phase_h(nc, G, int(ph[1]))
        elif ph[0] == "o":
            phase_o(nc, G, int(ph[1]), want_logits=(ph[1] == "1"))
        elif ph.startswith("ffn"):
            phase_ffn(nc, G, int(ph[3]), moe=(ph[3] == "1"))
        if ph == "ln0" or ph[0] == "o" or ph == "ffn0":
            G["Hsrc"] = G["H"]
            G["HTsrc"] = G["HT"]
        if ph == "o1":
            G["LGsrc"] = G["LG"]
    if "out" not in G and not dbg_out:
        pass
    return nc, list(G.used_inputs)


def host_inputs(inputs):
    rep = dict(host_consts())
    for k in WEIGHT_NAMES:
        a = np.asarray(inputs[k], dtype=np.float32)
        if a.ndim == 1:
            a = a[None, :]
        rep[k] = np.ascontiguousarray(a)
    ca = np.asarray(inputs["conv_a_w"], np.float32)
    chw = np.asarray(inputs["conv_h_w"], np.float32)
    chb = np.asarray(inputs["conv_h_b"], np.float32)
    cols = [ca[:, j, :] for j in range(3)]
    for k in range(3):
        cols += [chw[:, j, k * D:(k + 1) * D] for j in range(3)]
    cols += [chb[:, k * D:(k + 1) * D] for k in range(3)]
    cols += [np.zeros_like(cols[0])]
    prm = np.stack(cols, axis=-1)
    rep["cprm"] = np.ascontiguousarray(prm.reshape(2, 8, 128, 16))
    fq = np.asarray(inputs["flt_freq"], np.float32)
    rep["fprm"] = np.ascontiguousarray(np.stack([np.asarray(inputs["flt_b1"], np.float32), fq,
                                                 np.asarray(inputs["flt_b2"], np.float32), fq], axis=-1))
    return rep


LAUNCHES = [PHASES]
HANDOVER = {"H": "Hin", "HT": "HTin", "LG": "LGin"}


def kernel(**inputs):
    x = np.asarray(inputs["x"], dtype=np.float32)
    rep = host_inputs(inputs)
    xs = x.reshape(NCORES, T, D)
    carry = [dict() for _ in range(NCORES)]
    res = None
    for li, phases in enumerate(LAUNCHES):
        shapes = {k: (v.shape, v.dtype) for k, v in rep.items()}
        shapes["x"] = ((T, D), np.float32)
        for k, v in carry[0].items():
            shapes[k] = (v.shape, v.dtype)
        last = li == len(LAUNCHES) - 1
        outs = () if last else (("H", "HT", "LG") if "o1" in phases else ("H", "HT"))
        nc, used = build(shapes, phases=phases, dbg_out=outs)
        in_maps = []
        for c in range(NCORES):
            m = {}
            for k in used:
                if k == "x":
                    m[k] = np.ascontiguousarray(xs[c])
                elif k in carry[c]:
                    m[k] = carry[c][k]
                else:
                    m[k] = rep[k]
            in_maps.append(m)
        res = run_bass_kernel_spmd(nc, in_maps, core_ids=list(range(NCORES)))
        if not last:
            for c in range(NCORES):
                for k in outs:
                    carry[c][HANDOVER[k]] = np.asarray(res.results[c][k])
    out = np.stack([np.asarray(r["out"], dtype=np.float32) for r in res.results], axis=0)
    return out.reshape(16, L, D)
```

```python
import math
from contextlib import ExitStack

import numpy as np
import ml_dtypes
import concourse.bass as bass
import concourse.mybir as mybir
from concourse.bass_utils import run_bass_kernel_spmd

F32 = mybir.dt.float32
BF16 = mybir.dt.bfloat16
AF = mybir.ActivationFunctionType
ALU = mybir.AluOpType

NCORES = 8
D = 1024
L = 4096
NBC = 2
T = NBC * L
NFFT = 2 * L
DFF = 2816
DFE = 3584
NE = 8
ALPHA = (2 * 2) ** 0.25
LN_EPS = 1e-5
PI = math.pi


class Buf:
    __slots__ = ("w", "r", "multi")

    def __init__(self, multi=False):
        self.w = []
        self.r = []
        self.multi = multi


class Phase:
    ENG = ("pe", "act", "dve", "pool", "sp")

    def __init__(self, nc, name):
        self.nc = nc
        self.name = name
        self.es = ExitStack()
        self.ops = {e: [] for e in self.ENG}
        pool = getattr(nc, "_mk_sempool", None)
        if pool is None:
            pool = {"eng": {e: nc.alloc_semaphore(name=f"mk_{e}") for e in self.ENG},
                    "cnt": {e: 0 for e in self.ENG}, "dma": []}
            nc._mk_sempool = pool
        self.pool = pool
        self.sem = pool["eng"]
        self.cnt = pool["cnt"]
        self.dslot = {}
        self.waited = {e: {} for e in self.ENG}
        self.nal = 0

    def sb(self, shape, dt):
        self.nal += 1
        return self.es.enter_context(self.nc.sbuf_tensor(f"{self.name}_sb{self.nal}", list(shape), dt))

    def ps(self, shape, dt=F32):
        self.nal += 1
        return self.es.enter_context(self.nc.psum_tensor(f"{self.name}_ps{self.nal}", list(shape), dt))

    def _waits(self, eng, evs):
        best = {}
        for ev in evs:
            if ev is None:
                continue
            sem, val, key = ev
            if self.waited[eng].get(key, 0) >= val:
                continue
            if key not in best or best[key][1] < val:
                best[key] = (sem, val)
        out = []
        for key, (sem, val) in best.items():
            self.waited[eng][key] = val
            out.append((sem, val))
        return out

    def op(self, eng, fn, reads=(), writes=()):
        evs = []
        for b in reads:
            evs.extend(b.w)
        for b in writes:
            evs.extend(b.w)
            evs.extend(b.r)
        waits = self._waits(eng, evs)
        self.cnt[eng] += 1
        ev = (self.sem[eng], self.cnt[eng], eng)
        self.ops[eng].append((waits, fn, (self.sem[eng], 1)))
        for b in reads:
            b.r.append(ev)
        for b in writes:
            b.w = [ev]
            b.r = []
        return ev

    def dma(self, q, out, in_, reads=(), writes=(), key=None):
        kid = id(key)
        if kid not in self.dslot:
            i = len(self.dslot)
            if i >= len(self.pool["dma"]):
                self.pool["dma"].append([self.nc.alloc_semaphore(name=f"mk_d{i}"), 0])
            self.dslot[kid] = self.pool["dma"][i]
        slot = self.dslot[kid]
        sem = slot[0]
        dk = ("d", kid)
        evs = []
        for b in reads:
            evs.extend(b.w)
        for b in writes:
            if not b.multi:
                evs.extend(w for w in b.w if w[2] != dk)
            evs.extend(b.r)
        waits = self._waits(q, evs)
        slot[1] += 16
        ev = (sem, slot[1], dk)
        self.ops[q].append((waits, (lambda e, o=out, i=in_: e.dma_start(out=o, in_=i)), (sem, 16)))
        for b in reads:
            b.r.append(ev)
        for b in writes:
            if b.multi:
                b.w = [w for w in b.w if w[2] != dk] + [ev]
            else:
                b.w = [ev]
            b.r = []
        return ev

    def finish(self):
        nc = self.nc
        final = [(sl[0], sl[1]) for sl in self.dslot.values()]
        final += [(self.sem[e], self.cnt[e]) for e in self.ENG if self.cnt[e] > 0]
        ops = self.ops

        def replay(e, lst):
            for waits, fn, inc in lst:
                for sem, val in waits:
                    e.wait_ge(sem, val)
                fn(e).then_inc(inc[0], inc[1])

        with nc.Block() as block:
            @block.sync
            def _(e):
                replay(e, ops["sp"])
                for sem, val in final:
                    e.wait_ge(sem, val)

            @block.tensor
            def _(e):
                replay(e, ops["pe"])

            @block.scalar
            def _(e):
                replay(e, ops["act"])

            @block.vector
            def _(e):
                replay(e, ops["dve"])

            @block.gpsimd
            def _(e):
                replay(e, ops["pool"])
        self.es.close()


class Ring:
    def __init__(self, P, shape, dt, n, psum=False):
        self.t = [(P.ps(shape, dt) if psum else P.sb(shape, dt)) for _ in range(n)]
        self.b = [Buf() for _ in range(n)]
        self.i = 0

    def next(self):
        k = self.i % len(self.t)
        self.i += 1
        return self.t[k], self.b[k]


def load_const(P, shape, dt, src, q="sp"):
    t = P.sb(shape, dt)
    b = Buf()
    P.dma(q, t[:], src, writes=[b], key=b)
    return t, b


class LNCtx:
    def __init__(self, P, G, gam_row, bet_row, want_T=True, tp_ring=None):
        self.P = P
        self.gam, self.gamB = load_const(P, [128, D], F32, gam_row.partition_broadcast(128))
        self.bet, self.betB = load_const(P, [128, D], F32, bet_row.partition_broadcast(128))
        self.eps, self.epsB = load_const(P, [128, 1], F32, G["eps"])
        self.st = Ring(P, [128, 12], F32, 2)
        self.mv = Ring(P, [128, 4], F32, 2)
        self.want_T = want_T
        if want_T:
            self.idb, self.idbB = load_const(P, [128, 128], BF16, G["identb"])
            self.hb = Ring(P, [128, D], BF16, 2)
            self.tp = tp_ring if tp_ring is not None else Ring(P, [128, 8, 128], BF16, 2, psum=True)
            self.stage = Ring(P, [128, 8, 512], BF16, 2)

    def norm(self, r, rB, hn, hnB):
        P = self.P
        st, stB = self.st.next()
        mv, mvB = self.mv.next()

        def f1(e):
            e.bn_stats(st[:, 0:6], r[:, 0:512])
            return e.bn_stats(st[:, 6:12], r[:, 512:1024])
        P.op("dve", f1, reads=[rB], writes=[stB])
        P.op("dve", lambda e: e.bn_aggr(mv[:, 0:2], st[:, 0:12]), reads=[stB], writes=[mvB])
        P.op("act", lambda e: e.activation(mv[:, 2:3], mv[:, 1:2], AF.Sqrt, bias=self.eps[:, 0:1], scale=1.0),
             reads=[mvB, self.epsB], writes=[mvB])
        P.op("dve", lambda e: e.reciprocal(mv[:, 2:3], mv[:, 2:3]), reads=[mvB], writes=[mvB])
        P.op("dve", lambda e: e.scalar_tensor_tensor(out=mv[:, 3:4], in0=mv[:, 0:1], scalar=-1.0, in1=mv[:, 2:3],
                                                     op0=ALU.mult, op1=ALU.mult), reads=[mvB], writes=[mvB])
        P.op("act", lambda e: e.activation(hn[:], r[:], AF.Identity, bias=mv[:, 3:4], scale=mv[:, 2:3]),
             reads=[rB, mvB], writes=[hnB])
        P.op("dve", lambda e: e.tensor_tensor(out=hn[:], in0=hn[:], in1=self.gam[:], op=ALU.mult),
             reads=[hnB, self.gamB], writes=[hnB])
        P.op("pool", lambda e: e.tensor_tensor(out=hn[:], in0=hn[:], in1=self.bet[:], op=ALU.add),
             reads=[hnB, self.betB], writes=[hnB])

    def transpose_into(self, hn, hnB, stage, stageB, i):
        P = self.P
        hb, hbB = self.hb.next()
        tp, tpB = self.tp.next()
        P.op("act", lambda e: e.copy(hb[:], hn[:]), reads=[hnB], writes=[hbB])

        def ft(e):
            for j in range(8):
                ins = e.transpose(tp[:, j, :], hb[:, j * 128:(j + 1) * 128], self.idb[:])
            return ins
        P.op("pe", ft, reads=[hbB, self.idbB], writes=[tpB])
        P.op("dve", lambda e: e.tensor_copy(stage[:, :, i * 128:(i + 1) * 128], tp[:, :, :]),
             reads=[tpB], writes=[stageB])


def ht_view(G):
    return G["HT"].rearrange("(kc p) t -> p kc t", p=128)


def hts_view(G):
    return G["HTsrc"].rearrange("(kc p) t -> p kc t", p=128)


def phase_ln0(nc, G):
    P = Phase(nc, "ln0")
    C = LNCtx(P, G, G["ln_in_g"], G["ln_in_b"])
    xin = Ring(P, [128, D], F32, 3)
    hnr = Ring(P, [128, D], F32, 3)
    HTv = ht_view(G)
    dH = Buf(multi=True)
    dHT = Buf(multi=True)
    for g4 in range(T // 512):
        stage, stageB = C.stage.next()
        for i in range(4):
            t0 = (g4 * 4 + i) * 128
            r, rB = xin.next()
            hn, hnB = hnr.next()
            P.dma("sp", r[:], G["x"][t0:t0 + 128, :], writes=[rB], key=rB)
            C.norm(r, rB, hn, hnB)
            P.dma("pool", G["H"][t0:t0 + 128, :], hn[:], reads=[hnB], writes=[dH], key=hnB)
            C.transpose_into(hn, hnB, stage, stageB, i)
        P.dma("pool", HTv[:, :, g4 * 512:(g4 + 1) * 512], stage[:], reads=[stageB], writes=[dHT], key=stageB)
    P.finish()


def phase_a(nc, G, l):
    P = Phase(nc, f"a{l}")
    wv = G["w_in"][l].rearrange("(kc p) n -> p kc n", p=128)
    HTv = hts_view(G)
    idb, idbB = load_const(P, [128, 128], BF16, G["identb"])
    wring = Ring(P, [128, 8, 8, 128], BF16, 2)
    prmring = Ring(P, [128, 16], F32, 2)
    htring = Ring(P, [128, 8, 512], BF16, 3)
    psring = Ring(P, [128, 512], F32, 6, psum=True)
    tpring = Ring(P, [128, 8, 128], BF16, 2, psum=True)
    pj = [P.sb([128, L + 2], BF16) for _ in range(6)]
    pjB = [Buf() for _ in range(6)]
    for k in range(6):
        P.op("pool", lambda e, k=k: e.memset(pj[k][:], 0.0), writes=[pjB[k]])
    gst, gstB = P.sb([128, 2, L], BF16), Buf()
    tmp, tmpB = P.sb([128, L], F32), Buf()
    tv, tvB = P.sb([128, L], F32), Buf()
    ya_st, yaB = P.sb([128, L], BF16), Buf()
    x0_st, x0B = P.sb([128, L], BF16), Buf()
    z_st, zB = P.sb([128, L], BF16), Buf()
    zt_st, ztB = P.sb([128, 32, 128], BF16), Buf()
    dOut = Buf(multi=True)
    boff = [0, 1024, 2048, 3072, 4096, 5120, 6144, 7168]
    def load_unit(cc):
        wb, wbB = wring.next()
        for blk in range(8):
            c0 = boff[blk] + cc * 128
            P.dma("pool", wb[:, :, blk, :], wv[:, :, c0:c0 + 128], writes=[wbB], key=wbB)
        prm, prmB = prmring.next()
        P.dma("sp", prm[:], G["cprm"][l, cc], writes=[prmB], key=prmB)
        return wb, wbB, prm, prmB
    nxt = load_unit(0)
    for b in range(NBC):
        for cc in range(8):
            wb, wbB, prm, prmB = nxt
            if not (b == NBC - 1 and cc == 7):
                nxt = load_unit((cc + 1) % 8)
            for tt in range(8):
                ht, htB = htring.next()
                P.dma("sp", ht[:], HTv[:, :, b * L + tt * 512: b * L + (tt + 1) * 512], writes=[htB], key=htB)
                for blk in range(8):
                    ps, psB = psring.next()

                    def mm(e, ps=ps, wb=wb, ht=ht, blk=blk):
                        for kc in range(8):
                            ins = e.matmul(ps[:], wb[:, kc, blk, :], ht[:, kc, :], start=(kc == 0), stop=(kc == 7))
                        return ins
                    P.op("pe", mm, reads=[wbB, htB], writes=[psB])
                    if blk < 6:
                        P.op("act", lambda e, ps=ps, blk=blk, tt=tt: e.copy(pj[blk][:, 1 + tt * 512: 1 + (tt + 1) * 512], ps[:]),
                             reads=[psB], writes=[pjB[blk]])
                    else:
                        P.op("act", lambda e, ps=ps, blk=blk, tt=tt: e.activation(
                            gst[:, blk - 6, tt * 512:(tt + 1) * 512], ps[:], AF.Sigmoid), reads=[psB], writes=[gstB])
            c1 = slice(1, L + 1)
            cm = slice(0, L)
            cp = slice(2, L + 2)
            P.op("pool", lambda e: e.tensor_tensor(out=pj[1][:, c1], in0=pj[1][:, c1], in1=pj[2][:, c1], op=ALU.mult),
                 reads=[pjB[1], pjB[2]], writes=[pjB[1]])
            P.op("pool", lambda e, prm=prm: e.tensor_scalar(tmp[:], pj[1][:, cm], prm[:, 0:1], 0.0, op0=ALU.mult, op1=ALU.add),
                 reads=[pjB[1], prmB], writes=[tmpB])
            P.op("dve", lambda e, prm=prm: e.scalar_tensor_tensor(out=tmp[:], in0=pj[1][:, c1], scalar=prm[:, 1:2], in1=tmp[:],
                                                                  op0=ALU.mult, op1=ALU.add),
                 reads=[pjB[1], prmB, tmpB], writes=[tmpB])
            P.op("dve", lambda e, prm=prm: e.scalar_tensor_tensor(out=tmp[:], in0=pj[1][:, cp], scalar=prm[:, 2:3], in1=tmp[:],
                                                                  op0=ALU.mult, op1=ALU.add),
                 reads=[pjB[1], prmB, tmpB], writes=[tmpB])
            P.op("pool", lambda e: e.tensor_tensor(out=ya_st[:], in0=pj[0][:, c1], in1=tmp[:], op=ALU.mult),
                 reads=[pjB[0], tmpB], writes=[yaB])
            P.dma("pool", G["YA"][cc * 128:(cc + 1) * 128, b * L:(b + 1) * L], ya_st[:], reads=[yaB], writes=[dOut], key=yaB)

            def conv_h(src, srcB, k, dst, dstB):
                P.op("pool", lambda e, prm=prm: e.tensor_scalar(tmp[:] if dst is None else dst[:], src[:, cm],
                                                               prm[:, 3 + 3 * k:4 + 3 * k], prm[:, 12 + k:13 + k],
                                                               op0=ALU.mult, op1=ALU.add),
                     reads=[srcB, prmB], writes=[dstB])
            conv_h(pj[3], pjB[3], 0, tv, tvB)
            for j, sl in ((1, c1), (2, cp)):
                P.op("dve", lambda e, prm=prm, j=j, sl=sl: e.scalar_tensor_tensor(
                    out=tv[:], in0=pj[3][:, sl], scalar=prm[:, 3 + j:4 + j], in1=tv[:], op0=ALU.mult, op1=ALU.add),
                    reads=[pjB[3], prmB, tvB], writes=[tvB])
            conv_h(pj[4], pjB[4], 1, tmp, tmpB)
            for j, sl in ((1, c1), (2, cp)):
                P.op("dve", lambda e, prm=prm, j=j, sl=sl: e.scalar_tensor_tensor(
                    out=tmp[:], in0=pj[4][:, sl], scalar=prm[:, 6 + j:7 + j], in1=tmp[:], op0=ALU.mult, op1=ALU.add),
                    reads=[pjB[4], prmB, tmpB], writes=[tmpB])
            P.op("pool", lambda e: e.tensor_tensor(out=z_st[:], in0=tv[:], in1=tmp[:], op=ALU.mult),
                 reads=[tvB, tmpB], writes=[zB])
            conv_h(pj[5], pjB[5], 2, tmp, tmpB)
            P.op("dve", lambda e, prm=prm: e.scalar_tensor_tensor(
                out=tmp[:], in0=pj[5][:, c1], scalar=prm[:, 10:11], in1=tmp[:], op0=ALU.mult, op1=ALU.add),
                reads=[pjB[5], prmB, tmpB], writes=[tmpB])
            P.op("dve", lambda e, prm=prm: e.scalar_tensor_tensor(
                out=x0_st[:], in0=pj[5][:, cp], scalar=prm[:, 11:12], in1=tmp[:], op0=ALU.mult, op1=ALU.add),
                reads=[pjB[5], prmB, tmpB], writes=[x0B])
            P.dma("pool", G["X0T"][cc * 128:(cc + 1) * 128, b * L:(b + 1) * L], x0_st[:], reads=[x0B], writes=[dOut], key=x0B)
            for q in range(4):
                tp, tpB = tpring.next()

                def ft(e, tp=tp, q=q):
                    for j in range(8):
                        sc = q * 8 + j
                        ins = e.transpose(tp[:, j, :], z_st[:, sc * 128:(sc + 1) * 128], idb[:])
                    return ins
                P.op("pe", ft, reads=[zB, idbB], writes=[tpB])
                P.op("act", lambda e, tp=tp, q=q: e.copy(zt_st[:, q * 8:(q + 1) * 8, :], tp[:, :, :]), reads=[tpB], writes=[ztB])
            P.dma("pool", G["Z"][b, cc], zt_st[:].rearrange("p a c -> p (a c)"), reads=[ztB], writes=[dOut], key=ztB)
            for k in range(2):
                P.dma("pool", G["GT"][k * D + cc * 128: k * D + (cc + 1) * 128, b * L:(b + 1) * L], gst[:, k, :],
                      reads=[gstB], writes=[dOut], key=gstB)
    P.finish()


def phase_f(nc, G, l):
    P = Phase(nc, f"f{l}")
    posz, poszB = load_const(P, [33, L], F32, G["posz"])
    w1, w1B = load_const(P, [33, 64], F32, G["flt_w1"][l])
    w2, w2B = load_const(P, [64, 64], F32, G["flt_w2"][l])
    w3, w3B = load_const(P, [64, 2048], F32, G["flt_w3"][l])
    fp, fpB = load_const(P, [64, 4], F32, G["fprm"][l])
    tcol, tcolB = load_const(P, [128, 32], F32, G["tcol"])
    ndl, ndlB = load_const(P, [128, D], F32, G["negdelta"].partition_broadcast(128))
    hb_, hbB_ = load_const(P, [128, D], F32, G["hyena_bias"][l:l + 1, :].partition_broadcast(128))
    ones, onesB = load_const(P, [128, 128], F32, G["ones"])
    nyq, nyqB = load_const(P, [128, 32], BF16, G["nyqF"])
    eps6, eps6B = load_const(P, [128, 1], F32, G["eps6"])
    h1, h1B = P.sb([64, L], F32), Buf()
    h2, h2B = P.sb([64, L], F32), Buf()
    wr, wrB = P.sb([64, L], F32), Buf()
    psr = Ring(P, [128, 512], F32, 4, psum=True)
    ssq = [P.ps([128, 512], F32) for _ in range(4)]
    ssqB = [Buf() for _ in range(4)]
    dKR = Buf(multi=True)

    def sin_layer(src, srcB, w, wB, K, col, dst, dstB):
        for tt in range(8):
            ps, psB = psr.next()
            P.op("pe", lambda e, ps=ps, tt=tt: e.matmul(ps[0:64, :], w[0:K, :], src[0:K, tt * 512:(tt + 1) * 512],
                                                        start=True, stop=True), reads=[srcB, wB], writes=[psB])
            sl = slice(tt * 512, (tt + 1) * 512)
            P.op("dve", lambda e, ps=ps, sl=sl: e.tensor_scalar(dst[:, sl], ps[0:64, :], fp[:, col:col + 1], fp[:, col + 1:col + 2],
                                                                op0=ALU.add, op1=ALU.mult), reads=[psB, fpB], writes=[dstB])
        for _ in range(2):
            P.op("dve", lambda e: e.tensor_scalar(wr[:], dst[:], PI, -2 * PI, op0=ALU.is_gt, op1=ALU.mult), reads=[dstB], writes=[wrB])
            P.op("dve", lambda e: e.tensor_tensor(out=dst[:], in0=dst[:], in1=wr[:], op=ALU.add), reads=[dstB, wrB], writes=[dstB])
            P.op("dve", lambda e: e.tensor_scalar(wr[:], dst[:], -PI, 2 * PI, op0=ALU.is_lt, op1=ALU.mult), reads=[dstB], writes=[wrB])
            P.op("dve", lambda e: e.tensor_tensor(out=dst[:], in0=dst[:], in1=wr[:], op=ALU.add), reads=[dstB, wrB], writes=[dstB])
        P.op("dve", lambda e: e.tensor_scalar(dst[:], dst[:], 3.141592, -3.141592, op0=ALU.min, op1=ALU.max),
             reads=[dstB], writes=[dstB])
        P.op("act", lambda e: e.activation(dst[:], dst[:], AF.Sin), reads=[dstB], writes=[dstB])

    sin_layer(posz, poszB, w1, w1B, 33, 0, h1, h1B)
    sin_layer(h1, h1B, w2, w2B, 64, 2, h2, h2B)

    decr = Ring(P, [128, D], F32, 1)
    krr = Ring(P, [128, 512], F32, 2)
    sqr = Ring(P, [128, 512], F32, 2)
    for sc in range(32):
        dec, decB = decr.next()
        P.op("act", lambda e, dec=dec, sc=sc: e.activation(dec[:], ndl[:], AF.Exp, scale=tcol[:, sc:sc + 1]),
             reads=[ndlB, tcolB], writes=[decB])
        for q in range(4):
            ps, psB = psr.next()
            P.op("pe", lambda e, ps=ps, sc=sc, q=q: e.matmul(ps[:], h2[:, sc * 128:(sc + 1) * 128], w3[:, q * 512:(q + 1) * 512],
                                                             start=True, stop=True), reads=[h2B, w3B], writes=[psB])
            kr, krB = krr.next()
            ch0 = (q % 2) * 512
            P.op("dve", lambda e, ps=ps, kr=kr, dec=dec, ch0=ch0: e.tensor_tensor(out=kr[:], in0=ps[:], in1=dec[:, ch0:ch0 + 512],
                                                                                  op=ALU.mult), reads=[psB, decB], writes=[krB])
            P.dma("pool", G["KRAW"][sc * 128:(sc + 1) * 128, q * 512:(q + 1) * 512], kr[:], reads=[krB], writes=[dKR], key=krB)
            sq, sqB = sqr.next()
            P.op("act", lambda e, sq=sq, kr=kr: e.activation(sq[:], kr[:], AF.Square), reads=[krB], writes=[sqB])
            P.op("pe", lambda e, sq=sq, q=q, sc=sc: e.matmul(ssq[q][:], ones[:], sq[:], start=(sc == 0), stop=(sc == 31)),
                 reads=[sqB, onesB], writes=[ssqB[q]])
    rn, rnB = P.sb([128, 4, 512], F32), Buf()
    for q in range(4):
        P.op("act", lambda e, q=q: e.activation(rn[:, q, :], ssq[q][:], AF.Sqrt, bias=eps6[:, 0:1], scale=1.0),
             reads=[ssqB[q], eps6B], writes=[rnB])
    P.op("dve", lambda e: e.reciprocal(rn[:], rn[:]), reads=[rnB], writes=[rnB])

    A, AB = P.sb([128, 32, 512], BF16), Buf()
    Bm, BmB = P.sb([128, 32, 512], BF16), Buf()
    ldr = Ring(P, [128, 2, 512], F32, 2)
    fring = Ring(P, [128, 32, 128], BF16, 2)
    kor = Ring(P, [128, 512], F32, 2)
    dK = Buf(multi=True)
    for hh in range(2):
        for sc in range(32):
            ld, ldB = ldr.next()
            for d in range(2):
                q = d * 2 + hh
                P.dma("sp", ld[:, d, :], G["KRAW"][sc * 128:(sc + 1) * 128, q * 512:(q + 1) * 512], reads=[dKR], writes=[ldB], key=ldB)
            P.op("dve", lambda e, ld=ld, hh=hh: e.tensor_tensor(out=ld[:, 0, :], in0=ld[:, 0, :], in1=rn[:, hh, :], op=ALU.mult),
                 reads=[ldB, rnB], writes=[ldB])
            P.op("dve", lambda e, ld=ld, hh=hh: e.tensor_tensor(out=ld[:, 1, :], in0=ld[:, 1, :], in1=rn[:, 2 + hh, :], op=ALU.mult),
                 reads=[ldB, rnB], writes=[ldB])
            if sc == 0:
                P.op("dve", lambda e, ld=ld: e.memset(ld[0:1, 1, :], 0.0), reads=[ldB], writes=[ldB])
            P.op("pool", lambda e, ld=ld, sc=sc: e.tensor_tensor(out=A[:, sc, :], in0=ld[:, 0, :], in1=ld[:, 1, :], op=ALU.add),
                 reads=[ldB], writes=[AB])
            P.op("pool", lambda e, ld=ld, sc=sc: e.tensor_tensor(out=Bm[:, sc, :], in0=ld[:, 0, :], in1=ld[:, 1, :], op=ALU.subtract),
                 reads=[ldB], writes=[BmB])
        for fc in range(32):
            for kind in range(2):
                fb, fbB = fring.next()
                P.dma("sp", fb[:], G["dftF"][kind * 32 + fc], writes=[fbB], key=fbB)
                ps, psB = psr.next()
                src, srcB = (A, AB) if kind == 0 else (Bm, BmB)

                def mm(e, ps=ps, fb=fb, src=src):
                    for sc in range(32):
                        ins = e.matmul(ps[:], fb[:, sc, :], src[:, sc, :], start=(sc == 0), stop=(sc == 31))
                    return ins
                P.op("pe", mm, reads=[fbB, srcB], writes=[psB])
                ko, koB = kor.next()
                if kind == 0:
                    P.op("dve", lambda e, ko=ko, ps=ps, hh=hh: e.tensor_tensor(out=ko[:], in0=ps[:], in1=hb_[:, hh * 512:(hh + 1) * 512],
                                                                               op=ALU.add), reads=[psB, hbB_], writes=[koB])
                else:
                    P.op("act", lambda e, ko=ko, ps=ps: e.copy(ko[:], ps[:]), reads=[psB], writes=[koB])
                dst = G["KC"] if kind == 0 else G["KS"]
                P.dma("pool", dst[fc * 128:(fc + 1) * 128, hh * 512:(hh + 1) * 512], ko[:], reads=[koB], writes=[dK], key=koB)
        ps, psB = psr.next()

        def mmn(e, ps=ps):
            for sc in range(32):
                ins = e.matmul(ps[0:1, :], nyq[:, sc:sc + 1], A[:, sc, :], start=(sc == 0), stop=(sc == 31))
            return ins
        P.op("pe", mmn, reads=[nyqB, AB], writes=[psB])
        ko, koB = kor.next()
        P.op("dve", lambda e, ko=ko, ps=ps, hh=hh: e.tensor_tensor(out=ko[0:1, :], in0=ps[0:1, :], in1=hb_[0:1, hh * 512:(hh + 1) * 512],
                                                                   op=ALU.add), reads=[psB, hbB_], writes=[koB])
        P.dma("pool", G["KN"][0:1, hh * 512:(hh + 1) * 512], ko[0:1, :], reads=[koB], writes=[dK], key=koB)
    P.finish()


def phase_h(nc, G, l):
    P = Phase(nc, f"h{l}")
    nyqF, nyqFB = load_const(P, [128, 32], BF16, G["nyqF"])
    nyqG, nyqGB = load_const(P, [1, L], BF16, G["nyqG"])
    z_sb, zB = P.sb([128, 4, 32, 128], BF16), Buf()
    fring = Ring(P, [128, 32, 128], BF16, 3)
    kring = Ring(P, [128, 2, 512], F32, 2)
    tring = Ring(P, [128, 512], F32, 8)
    Y, YB = P.sb([128, 2, 32, 512], BF16), Buf()
    Yn, YnB = P.sb([1, 512], BF16), Buf()
    knt, kntB = P.sb([1, 512], F32), Buf()
    gring = Ring(P, [128, 16, 512], BF16, 2)
    x0r = Ring(P, [128, 512], BF16, 3)
    outr = Ring(P, [128, 512], BF16, 3)
    psz = Ring(P, [128, 512], F32, 4, psum=True)
    acc = [P.ps([128, 512], F32) for _ in range(4)]
    accB = [Buf() for _ in range(4)]
    dOut = Buf(multi=True)
    for b in range(NBC):
        for g in range(2):
            for j in range(4):
                P.dma("sp", z_sb[:, j, :, :].rearrange("p a c -> p (a c)"), G["Z"][b, g * 4 + j], writes=[zB], key=zB)
            P.dma("sp", knt[:], G["KN"][0:1, g * 512:(g + 1) * 512], writes=[kntB], key=kntB)
            for fc in range(32):
                kt, ktB = kring.next()
                P.dma("sp", kt[:, 0, :], G["KC"][fc * 128:(fc + 1) * 128, g * 512:(g + 1) * 512], writes=[ktB], key=ktB)
                P.dma("sp", kt[:, 1, :], G["KS"][fc * 128:(fc + 1) * 128, g * 512:(g + 1) * 512], writes=[ktB], key=ktB)
                zp = []
                for kind in range(2):
                    fb, fbB = fring.next()
                    P.dma("sp", fb[:], G["dftF"][kind * 32 + fc], writes=[fbB], key=fbB)
                    ps, psB = psz.next()

                    def mm(e, ps=ps, fb=fb):
                        for sc in range(32):
                            ins = e.matmul(ps[:], fb[:, sc, :], z_sb[:, :, sc, :], start=(sc == 0), stop=(sc == 31))
                        return ins
                    P.op("pe", mm, reads=[fbB, zB], writes=[psB])
                    zp.append((ps, psB))
                (zc, zcB), (zs, zsB) = zp
                t = [tring.next() for _ in range(4)]
                P.op("dve", lambda e, zc=zc, kt=kt, t=t: e.tensor_tensor(out=t[0][0][:], in0=zc[:], in1=kt[:, 0, :], op=ALU.mult),
                     reads=[zcB, ktB], writes=[t[0][1]])
                P.op("dve", lambda e, zs=zs, kt=kt, t=t: e.tensor_tensor(out=t[1][0][:], in0=zs[:], in1=kt[:, 1, :], op=ALU.mult),
                     reads=[zsB, ktB], writes=[t[1][1]])
                P.op("dve", lambda e, zc=zc, kt=kt, t=t: e.tensor_tensor(out=t[2][0][:], in0=zc[:], in1=kt[:, 1, :], op=ALU.mult),
                     reads=[zcB, ktB], writes=[t[2][1]])
                P.op("dve", lambda e, zs=zs, kt=kt, t=t: e.tensor_tensor(out=t[3][0][:], in0=zs[:], in1=kt[:, 0, :], op=ALU.mult),
                     reads=[zsB, ktB], writes=[t[3][1]])
                P.op("pool", lambda e, t=t, fc=fc: e.tensor_tensor(out=Y[:, 0, fc, :], in0=t[0][0][:], in1=t[1][0][:], op=ALU.subtract),
                     reads=[t[0][1], t[1][1]], writes=[YB])
                P.op("pool", lambda e, t=t, fc=fc: e.tensor_tensor(out=Y[:, 1, fc, :], in0=t[2][0][:], in1=t[3][0][:], op=ALU.add),
                     reads=[t[2][1], t[3][1]], writes=[YB])
            ps, psB = psz.next()

            def mmn(e, ps=ps):
                for sc in range(32):
                    ins = e.matmul(ps[0:1, :], nyqF[:, sc:sc + 1], z_sb[:, :, sc, :], start=(sc == 0), stop=(sc == 31))
                return ins
            P.op("pe", mmn, reads=[nyqFB, zB], writes=[psB])
            P.op("dve", lambda e, ps=ps: e.tensor_tensor(out=Yn[0:1, :], in0=ps[0:1, :], in1=knt[0:1, :], op=ALU.mult),
                 reads=[psB, kntB], writes=[YnB])
            for tt in range(8):
                for piece in range(4):
                    gp, gpB = gring.next()
                    P.dma("sp", gp[:], G["dftG"][tt, piece], writes=[gpB], key=gpB)
                    kind = piece // 2
                    for j in range(4):
                        def mmi(e, gp=gp, j=j, piece=piece, kind=kind, tt=tt):
                            for i in range(16):
                                fc = (piece % 2) * 16 + i
                                ins = e.matmul(acc[j][:], Y[:, kind, fc, j * 128:(j + 1) * 128], gp[:, i, :],
                                               start=(piece == 0 and i == 0), stop=False)
                            if piece == 3:
                                ins = e.matmul(acc[j][:], Yn[0:1, j * 128:(j + 1) * 128], nyqG[0:1, tt * 512:(tt + 1) * 512],
                                               start=False, stop=True)
                            return ins
                        rd = [gpB, YB] + ([YnB, nyqGB] if piece == 3 else [])
                        P.op("pe", mmi, reads=rd, writes=[accB[j]])
                for j in range(4):
                    ch0 = (g * 4 + j) * 128
                    x0t, x0B = x0r.next()
                    P.dma("sp", x0t[:], G["X0T"][ch0:ch0 + 128, b * L + tt * 512: b * L + (tt + 1) * 512], writes=[x0B], key=x0B)
                    ot, otB = outr.next()
                    P.op("dve", lambda e, ot=ot, j=j, x0t=x0t: e.tensor_tensor(out=ot[:], in0=acc[j][:], in1=x0t[:], op=ALU.mult),
                         reads=[accB[j], x0B], writes=[otB])
                    P.dma("pool", G["YH"][ch0:ch0 + 128, b * L + tt * 512: b * L + (tt + 1) * 512], ot[:], reads=[otB],
                          writes=[dOut], key=otB)
    P.finish()


def load_w_cast(P, dst, dstB, src_ap, nk, ncols, col0=0):
    v = src_ap.rearrange("(kc p) n -> p kc n", p=128)
    for kc in range(nk):
        for c in range(0, ncols, 2048):
            w = min(2048, ncols - c)
            P.dma("pool", dst[:, kc, c:c + w], v[:, kc, col0 + c:col0 + c + w], writes=[dstB], key=dstB)


def phase_o(nc, G, l, want_logits):
    P = Phase(nc, f"o{l}")
    C = LNCtx(P, G, G["ln_mix_g"][l:l + 1, :], G["ln_mix_b"][l:l + 1, :])
    wa, waB = P.sb([128, 8, D], BF16), Buf()
    wh, whB = P.sb([128, 8, D], BF16), Buf()
    wo, woB = P.sb([128, 8, D], BF16), Buf()
    load_w_cast(P, wa, waB, G["w_a_out"][l], 8, D)
    load_w_cast(P, wh, whB, G["w_h_out"][l], 8, D)
    load_w_cast(P, wo, woB, G["w_o"][l], 8, D)
    yar = Ring(P, [128, 8, 512], BF16, 2)
    yhr = Ring(P, [128, 8, 512], BF16, 2)
    gtr = Ring(P, [128, 16, 512], BF16, 2)
    mgr = Ring(P, [128, 8, 512], BF16, 2)
    t1r = Ring(P, [128, 512], F32, 2)
    t2r = Ring(P, [128, 512], F32, 2)
    hin = Ring(P, [128, D], F32, 3)
    rr = Ring(P, [128, D], F32, 2)
    hnr = Ring(P, [128, D], F32, 2)
    psp = Ring(P, [128, 512], F32, 4, psum=True)
    pso = Ring(P, [128, 512], F32, 2, psum=True)
    YAv = G["YA"].rearrange("(kc p) t -> p kc t", p=128)
    YHv = G["YH"].rearrange("(kc p) t -> p kc t", p=128)
    GTv = G["GT"].rearrange("(kc p) t -> p kc t", p=128)
    HTv = ht_view(G)
    dH, dHT = Buf(multi=True), Buf(multi=True)
    if want_logits:
        idf, idfB = load_const(P, [128, 128], F32, G["identf"])
        rt, rtB = load_const(P, [128, 8, NE], F32, G["moe_router"][0].rearrange("(kc p) e -> p kc e", p=128))
        hTf = Ring(P, [128, 8, 128], F32, 2)
        lgr = Ring(P, [128, NE], F32, 2)
        dLG = Buf(multi=True)
    for tt in range(T // 512):
        ts = slice(tt * 512, (tt + 1) * 512)
        ya, yaB = yar.next()
        yh, yhB = yhr.next()
        gt, gtB = gtr.next()
        P.dma("sp", ya[:], YAv[:, :, ts], writes=[yaB], key=yaB)
        P.dma("sp", yh[:], YHv[:, :, ts], writes=[yhB], key=yhB)
        P.dma("sp", gt[:], GTv[:, :, ts], writes=[gtB], key=gtB)
        mg, mgB = mgr.next()
        for j in range(8):
            pa, paB = psp.next()
            ph, phB = psp.next()

            def mma(e, pa=pa, ya=ya, j=j):
                for kc in range(8):
                    ins = e.matmul(pa[:], wa[:, kc, j * 128:(j + 1) * 128], ya[:, kc, :], start=(kc == 0), stop=(kc == 7))
                return ins

            def mmh(e, ph=ph, yh=yh, j=j):
                for kc in range(8):
                    ins = e.matmul(ph[:], wh[:, kc, j * 128:(j + 1) * 128], yh[:, kc, :], start=(kc == 0), stop=(kc == 7))
                return ins
            P.op("pe", mma, reads=[waB, yaB], writes=[paB])
            P.op("pe", mmh, reads=[whB, yhB], writes=[phB])
            t1, t1B = t1r.next()
            t2, t2B = t2r.next()
            P.op("dve", lambda e, t1=t1, pa=pa, gt=gt, j=j: e.tensor_tensor(out=t1[:], in0=pa[:], in1=gt[:, j, :], op=ALU.mult),
                 reads=[paB, gtB], writes=[t1B])
            P.op("dve", lambda e, t2=t2, ph=ph, gt=gt, j=j: e.tensor_tensor(out=t2[:], in0=ph[:], in1=gt[:, 8 + j, :], op=ALU.mult),
                 reads=[phB, gtB], writes=[t2B])
            P.op("pool", lambda e, t1=t1, t2=t2, mg=mg, j=j: e.tensor_tensor(out=mg[:, j, :], in0=t1[:], in1=t2[:], op=ALU.add),
                 reads=[t1B, t2B], writes=[mgB])
        stage, stageB = C.stage.next()
        for i in range(4):
            t0 = tt * 512 + i * 128
            hi, hiB = hin.next()
            P.dma("sp", hi[:], G["Hsrc"][t0:t0 + 128, :], writes=[hiB], key=hiB)
            r, rB = rr.next()
            for hf in range(2):
                po, poB = pso.next()

                def mmo(e, po=po, mg=mg, i=i, hf=hf):
                    for kc in range(8):
                        ins = e.matmul(po[:], mg[:, kc, i * 128:(i + 1) * 128], wo[:, kc, hf * 512:(hf + 1) * 512],
                                       start=(kc == 0), stop=(kc == 7))
                    return ins
                P.op("pe", mmo, reads=[mgB, woB], writes=[poB])
                P.op("dve", lambda e, r=r, hi=hi, po=po, hf=hf: e.scalar_tensor_tensor(
                    out=r[:, hf * 512:(hf + 1) * 512], in0=hi[:, hf * 512:(hf + 1) * 512], scalar=float(ALPHA), in1=po[:],
                    op0=ALU.mult, op1=ALU.add), reads=[hiB, poB], writes=[rB])
            hn, hnB = hnr.next()
            C.norm(r, rB, hn, hnB)
            P.dma("pool", G["H"][t0:t0 + 128, :], hn[:], reads=[hnB], writes=[dH], key=hnB)
            C.transpose_into(hn, hnB, stage, stageB, i)
            if want_logits:
                hT, hTB = hTf.next()
                for q in range(2):
                    pt, ptB = pso.next()

                    def ftf(e, pt=pt, hn=hn, q=q):
                        for jj in range(4):
                            j = q * 4 + jj
                            ins = e.transpose(pt[:, jj * 128:(jj + 1) * 128], hn[:, j * 128:(j + 1) * 128], idf[:])
                        return ins
                    P.op("pe", ftf, reads=[hnB, idfB], writes=[ptB])
                    P.op("act", lambda e, pt=pt, hT=hT, q=q: e.copy(hT[:, q * 4:(q + 1) * 4, :],
                                                                   pt[:].rearrange("p (a c) -> p a c", c=128)),
                         reads=[ptB], writes=[hTB])
                pl, plB = pso.next()

                def mml(e, pl=pl, hT=hT):
                    for kc in range(8):
                        ins = e.matmul(pl[:, 0:NE], hT[:, kc, :], rt[:, kc, :], start=(kc == 0), stop=(kc == 7))
                    return ins
                P.op("pe", mml, reads=[hTB, rtB], writes=[plB])
                lg, lgB = lgr.next()
                P.op("act", lambda e, lg=lg, pl=pl: e.copy(lg[:], pl[:, 0:NE]), reads=[plB], writes=[lgB])
                P.dma("pool", G["LG"][t0:t0 + 128, :], lg[:], reads=[lgB], writes=[dLG], key=lgB)
        P.dma("pool", HTv[:, :, ts], stage[:], reads=[stageB], writes=[dHT], key=stageB)
    P.finish()


def phase_ffn(nc, G, l, moe):
    P = Phase(nc, f"ffn{l}")
    TT = 1024 if moe else 512
    NH = TT // 512
    NS = TT // 128
    nff = (DFE if moe else DFF) // 128
    C = LNCtx(P, G, G["ln_ffn_g"][l:l + 1, :], G["ln_ffn_b"][l:l + 1, :], want_T=not moe)
    HTv = ht_view(G)
    HTs = hts_view(G)
    htr = Ring(P, [128, 8, TT], BF16, 2)
    aT, aTB = P.sb([128, nff, TT], BF16), Buf()
    w13r = Ring(P, [128, 2, 8, 128], BF16, 3)
    w2c = Ring(P, [128, 512], BF16, 4)
    sg = Ring(P, [128, 512], F32, 3)
    psg = Ring(P, [128, 512], F32, 8 if moe else 4, psum=True)
    hin = Ring(P, [128, D], F32, 3)
    accr = Ring(P, [128, D], F32, NS + 1)
    hnr = Ring(P, [128, D], F32, 2)
    dH, dHT = Buf(multi=True), Buf(multi=True)
    if moe:
        lgr = Ring(P, [128, NE], F32, NS + 1)
        cwr = Ring(P, [128, NE], F32, NS + 1)
        m8r = Ring(P, [128, 8], F32, 2)
        scr = Ring(P, [128, 3 * NE], F32, 2)
    nexp = NE if moe else 1
    for tt in range(T // TT):
        ts = slice(tt * TT, (tt + 1) * TT)
        ht, htB = htr.next()
        P.dma("sp", ht[:], HTs[:, :, ts], writes=[htB], key=htB)
        accs = []
        cws = []
        for i in range(TT // 128):
            t0 = tt * TT + i * 128
            hi, hiB = hin.next()
            P.dma("sp", hi[:], G["Hsrc"][t0:t0 + 128, :], writes=[hiB], key=hiB)
            ac, acB = accr.next()
            P.op("pool", lambda e, ac=ac, hi=hi: e.tensor_scalar(ac[:], hi[:], float(ALPHA), 0.0, op0=ALU.mult, op1=ALU.add),
                 reads=[hiB], writes=[acB])
            accs.append((ac, acB))
            if moe:
                lg, lgB = lgr.next()
                P.dma("sp", lg[:], G["LGsrc"][t0:t0 + 128, :], writes=[lgB], key=lgB)
                cw, cwB = cwr.next()
                m8, m8B = m8r.next()
                sc_, scB = scr.next()
                P.op("dve", lambda e, m8=m8, lg=lg: e.max(m8[:], lg[:]), reads=[lgB], writes=[m8B])
                P.op("dve", lambda e, sc_=sc_, lg=lg, m8=m8: e.tensor_scalar(sc_[:, 0:NE], lg[:], m8[:, 1:2], None, op0=ALU.is_ge),
                     reads=[lgB, m8B], writes=[scB])
                P.op("dve", lambda e, sc_=sc_, m8=m8: e.tensor_scalar(sc_[:, 2 * NE:2 * NE + 1], m8[:, 0:1], -1.0, None, op0=ALU.mult),
                     reads=[m8B, scB], writes=[scB])
                P.op("act", lambda e, sc_=sc_, lg=lg: e.activation(sc_[:, NE:2 * NE], lg[:], AF.Exp, bias=sc_[:, 2 * NE:2 * NE + 1], scale=1.0),
                     reads=[lgB, scB], writes=[scB])
                P.op("dve", lambda e, sc_=sc_: e.tensor_tensor(out=sc_[:, NE:2 * NE], in0=sc_[:, NE:2 * NE], in1=sc_[:, 0:NE], op=ALU.mult),
                     reads=[scB], writes=[scB])
                P.op("dve", lambda e, sc_=sc_: e.tensor_reduce(out=sc_[:, 2 * NE + 1:2 * NE + 2], in_=sc_[:, NE:2 * NE],
                                                               axis=mybir.AxisListType.X, op=ALU.add), reads=[scB], writes=[scB])
                P.op("dve", lambda e, sc_=sc_: e.reciprocal(sc_[:, 2 * NE + 1:2 * NE + 2], sc_[:, 2 * NE + 1:2 * NE + 2]),
                     reads=[scB], writes=[scB])
                P.op("dve", lambda e, sc_=sc_, cw=cw: e.tensor_scalar(cw[:], sc_[:, NE:2 * NE], sc_[:, 2 * NE + 1:2 * NE + 2], None, op0=ALU.mult),
                     reads=[scB], writes=[cwB])
                cws.append((cw, cwB))
        for ex in range(nexp):
            if moe:
                W1, W3, W2 = G["moe_w1"][0, ex], G["moe_w3"][0, ex], G["moe_w2"][0, ex]
            else:
                W1, W3, W2 = G["ffn_w1"][0], G["ffn_w3"][0], G["ffn_w2"][0]
            w1v = W1.rearrange("(kc p) n -> p kc n", p=128)
            w3v = W3.rearrange("(kc p) n -> p kc n", p=128)
            for c in range(nff):
                w13, w13B = w13r.next()
                P.dma("pool", w13[:, 0, :, :], w1v[:, :, c * 128:(c + 1) * 128], writes=[w13B], key=w13B)
                P.dma("pool", w13[:, 1, :, :], w3v[:, :, c * 128:(c + 1) * 128], writes=[w13B], key=w13B)
                for hh in range(NH):
                    pg, pgB = psg.next()
                    pu, puB = psg.next()
                    hsl = slice(hh * 512, (hh + 1) * 512)

                    def mmg(e, pg=pg, w13=w13, ht=ht, which=0, hsl=hsl):
                        for kc in range(8):
                            ins = e.matmul(pg[:], w13[:, which, kc, :], ht[:, kc, hsl], start=(kc == 0), stop=(kc == 7))
                        return ins
                    P.op("pe", mmg, reads=[w13B, htB], writes=[pgB])
                    P.op("pe", lambda e, pu=pu, w13=w13, ht=ht, hsl=hsl, mmg=mmg: mmg(e, pu, w13, ht, 1, hsl), reads=[w13B, htB], writes=[puB])
                    s_, sB = sg.next()
                    P.op("act", lambda e, s_=s_, pg=pg: e.activation(s_[:], pg[:], AF.Silu), reads=[pgB], writes=[sB])
                    P.op("dve", lambda e, s_=s_, pu=pu, c=c, hsl=hsl: e.tensor_tensor(out=aT[:, c, hsl], in0=pu[:], in1=s_[:], op=ALU.mult),
                         reads=[puB, sB], writes=[aTB])
            for hf in range(2):
                pyl = [psg.next() for _ in range(TT // 128)]
                for c in range(nff):
                    w2t, w2B = w2c.next()
                    P.dma("pool", w2t[:, 0:512], W2[c * 128:(c + 1) * 128, hf * 512:(hf + 1) * 512], writes=[w2B], key=w2B)
                    for i in range(TT // 128):
                        py, pyB = pyl[i]
                        P.op("pe", lambda e, py=py, i=i, c=c, w2t=w2t: e.matmul(py[:], aT[:, c, i * 128:(i + 1) * 128], w2t[:, 0:512],
                                                                               start=(c == 0), stop=(c == nff - 1)),
                             reads=[aTB, w2B], writes=[pyB])
                for i in range(TT // 128):
                    py, pyB = pyl[i]
                    ac, acB = accs[i]
                    hs = slice(hf * 512, (hf + 1) * 512)
                    if moe:
                        cw, cwB = cws[i]
                        P.op("dve", lambda e, ac=ac, py=py, cw=cw, ex=ex, hs=hs: e.scalar_tensor_tensor(
                            out=ac[:, hs], in0=py[:], scalar=cw[:, ex:ex + 1], in1=ac[:, hs], op0=ALU.mult, op1=ALU.add),
                            reads=[pyB, cwB, acB], writes=[acB])
                    else:
                        P.op("dve", lambda e, ac=ac, py=py, hs=hs: e.tensor_tensor(out=ac[:, hs], in0=py[:], in1=ac[:, hs], op=ALU.add),
                             reads=[pyB, acB], writes=[acB])
        if not moe:
            stage, stageB = C.stage.next()
        for i in range(TT // 128):
            t0 = tt * TT + i * 128
            ac, acB = accs[i]
            hn, hnB = hnr.next()
            C.norm(ac, acB, hn, hnB)
            if moe:
                P.dma("act", G["out"][t0:t0 + 128, :], hn[:], reads=[hnB], writes=[dH], key=hnB)
            else:
                P.dma("act", G["H"][t0:t0 + 128, :], hn[:], reads=[hnB], writes=[dH], key=hnB)
                C.transpose_into(hn, hnB, stage, stageB, i)
        if not moe:
            P.dma("act", HTv[:, :, ts], stage[:], reads=[stageB], writes=[dHT], key=stageB)
    P.finish()


_CONST = {}


def host_consts():
    if _CONST:
        return _CONST
    bf = ml_dtypes.bfloat16
    s = np.arange(L, dtype=np.float64)
    f = np.arange(L, dtype=np.float64)
    ang = 2.0 * np.pi * np.outer(s, f) / NFFT
    Fm = np.concatenate([np.cos(ang), np.sin(ang)], axis=1)
    Fb = Fm.reshape(32, 128, 64, 128).transpose(2, 1, 0, 3)
    _CONST["dftF"] = np.ascontiguousarray(Fb).astype(bf)
    Gc = (2.0 / NFFT) * np.cos(ang.T)
    Gc[0, :] = 1.0 / NFFT
    Gs = (2.0 / NFFT) * np.sin(ang.T)
    Gm = np.concatenate([Gc, Gs], axis=0)
    Gb = Gm.reshape(4, 16, 128, 8, 512).transpose(3, 0, 2, 1, 4)
    _CONST["dftG"] = np.ascontiguousarray(Gb).astype(bf)
    sgn = np.where(np.arange(128) % 2 == 0, 1.0, -1.0)
    _CONST["nyqF"] = np.ascontiguousarray(np.repeat(sgn[:, None], 32, axis=1)).astype(bf)
    _CONST["nyqG"] = ((1.0 / NFFT) * np.where(np.arange(L) % 2 == 0, 1.0, -1.0))[None, :].astype(bf)
    t = np.linspace(0.0, 1.0, L, dtype=np.float32)[:, None]
    bands = np.linspace(1e-4, 16 - 1, 16, dtype=np.float32)[None, :]
    w = (np.float32(2.0 * math.pi / L) * np.arange(L, dtype=np.float32))[:, None]
    angp = bands * w
    z = np.concatenate([t, np.cos(angp), -np.sin(angp)], axis=-1).astype(np.float32)
    _CONST["posz"] = np.ascontiguousarray(z.T)
    _CONST["tcol"] = np.ascontiguousarray(t[:, 0].reshape(32, 128).T)
    max_decay = math.log(1e-2) / 0.3
    min_decay = math.log(1e-2) / 1.5
    deltas = np.linspace(min_decay, max_decay, D, dtype=np.float32)
    _CONST["negdelta"] = (-np.abs(deltas))[None, :].astype(np.float32)
    _CONST["identb"] = np.eye(128, dtype=np.float32).astype(bf)
    _CONST["identf"] = np.eye(128, dtype=np.float32)
    _CONST["ones"] = np.ones((128, 128), np.float32)
    _CONST["eps"] = np.full((128, 1), LN_EPS, np.float32)
    _CONST["eps6"] = np.full((128, 1), 1e-6, np.float32)
    return _CONST


WEIGHT_NAMES = ["ln_in_g", "ln_in_b", "w_in", "flt_w1", "flt_w2", "flt_w3", "hyena_bias", "w_a_out", "w_h_out", "w_o",
                "ln_mix_g", "ln_mix_b", "ffn_w1", "ffn_w3", "ffn_w2", "moe_router", "moe_w1", "moe_w3", "moe_w2",
                "ln_ffn_g", "ln_ffn_b"]

SCRATCH = {
    "H": ([T, D], F32), "HT": ([D, T], BF16), "YA": ([D, T], BF16), "X0T": ([D, T], BF16), "GT": ([2 * D, T], BF16),
    "Z": ([NBC, 8, 128, L], BF16), "KRAW": ([L, 2 * D], F32), "KC": ([L, D], F32), "KS": ([L, D], F32), "KN": ([1, D], F32),
    "YH": ([D, T], BF16), "LG": ([T, NE], F32),
}

PHASES = ["ln0", "a0", "f0", "h0", "o0", "ffn0", "a1", "f1", "h1", "o1", "ffn1"]


class LazyG(dict):
    def __init__(self, nc, shapes, outs):
        super().__init__()
        self.nc, self.shapes, self.outs = nc, shapes, outs
        self.used_inputs = []

    def __missing__(self, name):
        nc = self.nc
        if name in ("Hsrc", "HTsrc", "LGsrc"):
            base = name[:-3]
            ap = self[base + "in"] if (base + "in") in self.shapes else self[base]
        elif name in self.shapes:
            shape, dt = self.shapes[name]
            bdt = BF16 if dt == ml_dtypes.bfloat16 else F32
            ap = nc.dram_tensor(name, list(shape), bdt, kind="ExternalInput").ap()
            self.used_inputs.append(name)
        elif name == "out":
            ap = nc.dram_tensor("out", [T, D], F32, kind="ExternalOutput").ap()
        else:
            shape, dt = SCRATCH[name]
            kind = "ExternalOutput" if name in self.outs else "Internal"
            ap = nc.dram_tensor(name, list(shape), dt, kind=kind).ap()
        self[name] = ap
        return ap


def build(shapes, phases=None, dbg_out=()):
    nc = bass.Bass("TRN2", target_bir_lowering=False)
    G = LazyG(nc, shapes, set(dbg_out))
    phases = PHASES if phases is None else phases
    for ph in phases:
        if ph == "ln0":
            phase_ln0(nc, G)
        elif ph[0] == "a":
            phase_a(nc, G, int(ph[1]))
        elif ph[0] == "f" and ph[1] != "f":
            phase_f(nc, G, int(ph[1]))
        elif ph[0] == "h":
            phase_h(nc, G, int(ph[1]))
        elif ph[0] == "o":
            phase_o(nc, G, int(ph[1]), want_logits=(ph[1] == "1"))
        elif ph.startswith("ffn"):
            phase_ffn(nc, G, int(ph[3]), moe=(ph[3] == "1"))
        if ph == "ln0" or ph[0] == "o" or ph == "ffn0":
            G["Hsrc"] = G["H"]
            G["HTsrc"] = G["HT"]
        if ph == "o1":
            G["LGsrc"] = G["LG"]
    if "out" not in G and not dbg_out:
        pass
    return nc, list(G.used_inputs)


def host_inputs(inputs):
    rep = dict(host_consts())
    for k in WEIGHT_NAMES:
        a = np.asarray(inputs[k], dtype=np.float32)
        if a.ndim == 1:
            a = a[None, :]
        rep[k] = np.ascontiguousarray(a)
    ca = np.asarray(inputs["conv_a_w"], np.float32)
    chw = np.asarray(inputs["conv_h_w"], np.float32)
    chb = np.asarray(inputs["conv_h_b"], np.float32)
    cols = [ca[:, j, :] for j in range(3)]
    for k in range(3):
        cols += [chw[:, j, k * D:(k + 1) * D] for j in range(3)]
    cols += [chb[:, k * D:(k + 1) * D] for k in range(3)]
    cols += [np.zeros_like(cols[0])]
    prm = np.stack(cols, axis=-1)
    rep["cprm"] = np.ascontiguousarray(prm.reshape(2, 8, 128, 16))
    fq = np.asarray(inputs["flt_freq"], np.float32)
    rep["fprm"] = np.ascontiguousarray(np.stack([np.asarray(inputs["flt_b1"], np.float32), fq,
                                                 np.asarray(inputs["flt_b2"], np.float32), fq], axis=-1))
    return rep


LAUNCHES = [PHASES]
HANDOVER = {"H": "Hin", "HT": "HTin", "LG": "LGin"}


def kernel(**inputs):
    x = np.asarray(inputs["x"], dtype=np.float32)
    rep = host_inputs(inputs)
    xs = x.reshape(NCORES, T, D)
    carry = [dict() for _ in range(NCORES)]
    res = None
    for li, phases in enumerate(LAUNCHES):
        shapes = {k: (v.shape, v.dtype) for k, v in rep.items()}
        shapes["x"] = ((T, D), np.float32)
        for k, v in carry[0].items():
            shapes[k] = (v.shape, v.dtype)
        last = li == len(LAUNCHES) - 1
        outs = () if last else (("H", "HT", "LG") if "o1" in phases else ("H", "HT"))
        nc, used = build(shapes, phases=phases, dbg_out=outs)
        in_maps = []
        for c in range(NCORES):
            m = {}
            for k in used:
                if k == "x":
                    m[k] = np.ascontiguousarray(xs[c])
                elif k in carry[c]:
                    m[k] = carry[c][k]
                else:
                    m[k] = rep[k]
            in_maps.append(m)
        res = run_bass_kernel_spmd(nc, in_maps, core_ids=list(range(NCORES)))
        if not last:
            for c in range(NCORES):
                for k in outs:
                    carry[c][HANDOVER[k]] = np.asarray(res.results[c][k])
    out = np.stack([np.asarray(r["out"], dtype=np.float32) for r in res.results], axis=0)
    return out.reshape(16, L, D)
```

```python
import math
import re
from contextlib import ExitStack

import numpy as np
import ml_dtypes
import concourse.bass as bass
import concourse.mybir as mybir
from concourse.bass_utils import run_bass_kernel_spmd

F32 = mybir.dt.float32
BF16 = mybir.dt.bfloat16
I32 = mybir.dt.int32
AF = mybir.ActivationFunctionType
ALU = mybir.AluOpType

NCORES = 8
D = 1024
L = 4096
NBC = 2
T = NBC * L
NFFT = 2 * L
DFF = 2816
DFE = 3584
NE = 8
ALPHA = (2 * 2) ** 0.25
DEBUG_DYN = False
NT = 23
NSLOT = NT * 1024
NFC = 28
LN_EPS = 1e-5
PI = math.pi


class Buf:
    __slots__ = ("w", "r", "multi")

    def __init__(self, multi=False):
        self.w = []
        self.r = []
        self.multi = multi


class Phase:
    ENG = ("pe", "act", "dve", "pool", "sp")

    def __init__(self, nc, name):
        self.nc = nc
        self.name = name
        self.es = ExitStack()
        self.ops = {e: [] for e in self.ENG}
        pool = getattr(nc, "_mk_sempool", None)
        if pool is None:
            pool = {"eng": {e: nc.alloc_semaphore(name=f"mk_{e}") for e in self.ENG},
                    "cnt": {e: 0 for e in self.ENG}, "dma": []}
            nc._mk_sempool = pool
        self.pool = pool
        self.sem = pool["eng"]
        self.cnt = pool["cnt"]
        self.dslot = {}
        self.waited = {e: {} for e in self.ENG}
        self.nal = 0
        self._bc = None

    def bcreg(self, e, val=None):
        val = NSLOT - 1 if val is None else val
        if self._bc is None:
            self._bc = {}
        if val not in self._bc:
            r = e.alloc_register(f"{self.name}_bc{val}")
            e.reg_mov(r, val)
            self._bc[val] = r
        return self._bc[val]

    def sb(self, shape, dt):
        self.nal += 1
        return self.es.enter_context(self.nc.sbuf_tensor(f"{self.name}_sb{self.nal}", list(shape), dt))

    def ps(self, shape, dt=F32):
        self.nal += 1
        return self.es.enter_context(self.nc.psum_tensor(f"{self.name}_ps{self.nal}", list(shape), dt))

    def _waits(self, eng, evs):
        best = {}
        for ev in evs:
            if ev is None:
                continue
            sem, val, key = ev
            if self.waited[eng].get(key, 0) >= val:
                continue
            if key not in best or best[key][1] < val:
                best[key] = (sem, val)
        out = []
        for key, (sem, val) in best.items():
            self.waited[eng][key] = val
            out.append((sem, val))
        return out

    def op(self, eng, fn, reads=(), writes=()):
        evs = []
        for b in reads:
            evs.extend(b.w)
        for b in writes:
            evs.extend(b.w)
            evs.extend(b.r)
        waits = self._waits(eng, evs)
        self.cnt[eng] += 1
        ev = (self.sem[eng], self.cnt[eng], eng)
        self.ops[eng].append((waits, fn, (self.sem[eng], 1)))
        for b in reads:
            b.r.append(ev)
        for b in writes:
            b.w = [ev]
            b.r = []
        return ev

    def dma(self, q, out, in_, reads=(), writes=(), key=None, fn=None):
        kid = id(key)
        if kid not in self.dslot:
            i = len(self.dslot)
            if i >= len(self.pool["dma"]):
                self.pool["dma"].append([self.nc.alloc_semaphore(name=f"mk_d{i}"), 0])
            self.dslot[kid] = self.pool["dma"][i]
        slot = self.dslot[kid]
        sem = slot[0]
        dk = ("d", kid)
        evs = []
        for b in reads:
            evs.extend(b.w)
        for b in writes:
            if not b.multi:
                evs.extend(w for w in b.w if w[2] != dk)
            evs.extend(b.r)
        waits = self._waits(q, evs)
        slot[1] += 16
        ev = (sem, slot[1], dk)
        if fn is None:
            fn = (lambda e, o=out, i=in_: e.dma_start(out=o, in_=i))
        self.ops[q].append((waits, fn, (sem, 16)))
        for b in reads:
            b.r.append(ev)
        for b in writes:
            if b.multi:
                b.w = [w for w in b.w if w[2] != dk] + [ev]
            else:
                b.w = [ev]
            b.r = []
        return ev

    def finish(self):
        nc = self.nc
        final = [(sl[0], sl[1]) for sl in self.dslot.values()]
        final += [(self.sem[e], self.cnt[e]) for e in self.ENG if self.cnt[e] > 0]
        ops = self.ops

        def replay(e, lst):
            for waits, fn, inc in lst:
                for sem, val in waits:
                    e.wait_ge(sem, val)
                fn(e).then_inc(inc[0], inc[1])

        with nc.Block() as block:
            @block.sync
            def _(e):
                replay(e, ops["sp"])
                for sem, val in final:
                    e.wait_ge(sem, val)

            @block.tensor
            def _(e):
                replay(e, ops["pe"])

            @block.scalar
            def _(e):
                replay(e, ops["act"])

            @block.vector
            def _(e):
                replay(e, ops["dve"])

            @block.gpsimd
            def _(e):
                replay(e, ops["pool"])
        self.es.close()


class Ring:
    def __init__(self, P, shape, dt, n, psum=False):
        self.t = [(P.ps(shape, dt) if psum else P.sb(shape, dt)) for _ in range(n)]
        self.b = [Buf() for _ in range(n)]
        self.i = 0

    def next(self):
        k = self.i % len(self.t)
        self.i += 1
        return self.t[k], self.b[k]


def load_const(P, shape, dt, src, q="sp"):
    t = P.sb(shape, dt)
    b = Buf()
    P.dma(q, t[:], src, writes=[b], key=b)
    return t, b


class LNCtx:
    def __init__(self, P, G, gam_row, bet_row, want_T=True, tp_ring=None):
        self.P = P
        self.gam, self.gamB = load_const(P, [128, D], F32, gam_row.partition_broadcast(128))
        self.bet, self.betB = load_const(P, [128, D], F32, bet_row.partition_broadcast(128))
        self.eps, self.epsB = load_const(P, [128, 1], F32, G["eps"])
        self.st = Ring(P, [128, 12], F32, 2)
        self.mv = Ring(P, [128, 4], F32, 2)
        self.want_T = want_T
        if want_T:
            self.idb, self.idbB = load_const(P, [128, 128], BF16, G["identb"])
            self.hb = Ring(P, [128, D], BF16, 2)
            self.tp = tp_ring if tp_ring is not None else Ring(P, [128, 8, 128], BF16, 2, psum=True)
            self.stage = Ring(P, [128, 8, 512], BF16, 2)

    def norm(self, r, rB, hn, hnB):
        P = self.P
        st, stB = self.st.next()
        mv, mvB = self.mv.next()

        def f1(e):
            e.bn_stats(st[:, 0:6], r[:, 0:512])
            return e.bn_stats(st[:, 6:12], r[:, 512:1024])
        P.op("dve", f1, reads=[rB], writes=[stB])
        P.op("dve", lambda e: e.bn_aggr(mv[:, 0:2], st[:, 0:12]), reads=[stB], writes=[mvB])
        P.op("act", lambda e: e.activation(mv[:, 2:3], mv[:, 1:2], AF.Sqrt, bias=self.eps[:, 0:1], scale=1.0),
             reads=[mvB, self.epsB], writes=[mvB])
        P.op("dve", lambda e: e.reciprocal(mv[:, 2:3], mv[:, 2:3]), reads=[mvB], writes=[mvB])
        P.op("dve", lambda e: e.scalar_tensor_tensor(out=mv[:, 3:4], in0=mv[:, 0:1], scalar=-1.0, in1=mv[:, 2:3],
                                                     op0=ALU.mult, op1=ALU.mult), reads=[mvB], writes=[mvB])
        P.op("act", lambda e: e.activation(hn[:], r[:], AF.Identity, bias=mv[:, 3:4], scale=mv[:, 2:3]),
             reads=[rB, mvB], writes=[hnB])
        P.op("dve", lambda e: e.tensor_tensor(out=hn[:], in0=hn[:], in1=self.gam[:], op=ALU.mult),
             reads=[hnB, self.gamB], writes=[hnB])
        P.op("pool", lambda e: e.tensor_tensor(out=hn[:], in0=hn[:], in1=self.bet[:], op=ALU.add),
             reads=[hnB, self.betB], writes=[hnB])

    def transpose_into(self, hn, hnB, stage, stageB, i):
        P = self.P
        hb, hbB = self.hb.next()
        tp, tpB = self.tp.next()
        P.op("act", lambda e: e.copy(hb[:], hn[:]), reads=[hnB], writes=[hbB])

        def ft(e):
            for j in range(8):
                ins = e.transpose(tp[:, j, :], hb[:, j * 128:(j + 1) * 128], self.idb[:])
            return ins
        P.op("pe", ft, reads=[hbB, self.idbB], writes=[tpB])
        P.op("dve", lambda e: e.tensor_copy(stage[:, :, i * 128:(i + 1) * 128], tp[:, :, :]),
             reads=[tpB], writes=[stageB])


def ht_view(G):
    return G["HT"].rearrange("(kc p) t -> p kc t", p=128)


def hts_view(G):
    return G["HTsrc"].rearrange("(kc p) t -> p kc t", p=128)


def phase_ln0(nc, G):
    P = Phase(nc, "ln0")
    C = LNCtx(P, G, G["ln_in_g"], G["ln_in_b"])
    xin = Ring(P, [128, D], F32, 3)
    hnr = Ring(P, [128, D], F32, 3)
    HTv = ht_view(G)
    dH = Buf(multi=True)
    dHT = Buf(multi=True)
    for g4 in range(T // 512):
        stage, stageB = C.stage.next()
        for i in range(4):
            t0 = (g4 * 4 + i) * 128
            r, rB = xin.next()
            hn, hnB = hnr.next()
            P.dma("sp", r[:], G["x"][t0:t0 + 128, :], writes=[rB], key=rB)
            C.norm(r, rB, hn, hnB)
            P.dma("pool", G["H"][t0:t0 + 128, :], hn[:], reads=[hnB], writes=[dH], key=hnB)
            C.transpose_into(hn, hnB, stage, stageB, i)
        P.dma("pool", HTv[:, :, g4 * 512:(g4 + 1) * 512], stage[:], reads=[stageB], writes=[dHT], key=stageB)
    P.finish()


def phase_a(nc, G, l):
    P = Phase(nc, f"a{l}")
    wv = G["w_in"][l].rearrange("(kc p) n -> p kc n", p=128)
    HTv = hts_view(G)
    idb, idbB = load_const(P, [128, 128], BF16, G["identb"])
    wring = Ring(P, [128, 8, 8, 128], BF16, 2)
    prmring = Ring(P, [128, 16], F32, 2)
    htring = Ring(P, [128, 8, 512], BF16, 3)
    psring = Ring(P, [128, 512], F32, 6, psum=True)
    tpring = Ring(P, [128, 8, 128], BF16, 2, psum=True)
    pj = [P.sb([128, L + 2], BF16) for _ in range(6)]
    pjB = [Buf() for _ in range(6)]
    for k in range(6):
        P.op("pool", lambda e, k=k: e.memset(pj[k][:], 0.0), writes=[pjB[k]])
    gst, gstB = P.sb([128, 2, L], BF16), Buf()
    tmp, tmpB = P.sb([128, L], F32), Buf()
    tv, tvB = P.sb([128, L], F32), Buf()
    ya_st, yaB = P.sb([128, L], BF16), Buf()
    x0_st, x0B = P.sb([128, L], BF16), Buf()
    z_st, zB = P.sb([128, L], BF16), Buf()
    zt_st, ztB = P.sb([128, 32, 128], BF16), Buf()
    dOut = Buf(multi=True)
    boff = [0, 1024, 2048, 3072, 4096, 5120, 6144, 7168]
    def load_unit(cc):
        wb, wbB = wring.next()
        for blk in range(8):
            c0 = boff[blk] + cc * 128
            P.dma("pool", wb[:, :, blk, :], wv[:, :, c0:c0 + 128], writes=[wbB], key=wbB)
        prm, prmB = prmring.next()
        P.dma("sp", prm[:], G["cprm"][l, cc], writes=[prmB], key=prmB)
        return wb, wbB, prm, prmB
    nxt = load_unit(0)
    for b in range(NBC):
        for cc in range(8):
            wb, wbB, prm, prmB = nxt
            if not (b == NBC - 1 and cc == 7):
                nxt = load_unit((cc + 1) % 8)
            for tt in range(8):
                ht, htB = htring.next()
                P.dma("sp", ht[:], HTv[:, :, b * L + tt * 512: b * L + (tt + 1) * 512], writes=[htB], key=htB)
                for blk in range(8):
                    ps, psB = psring.next()

                    def mm(e, ps=ps, wb=wb, ht=ht, blk=blk):
                        for kc in range(8):
                            ins = e.matmul(ps[:], wb[:, kc, blk, :], ht[:, kc, :], start=(kc == 0), stop=(kc == 7))
                        return ins
                    P.op("pe", mm, reads=[wbB, htB], writes=[psB])
                    if blk < 6:
                        P.op("act", lambda e, ps=ps, blk=blk, tt=tt: e.copy(pj[blk][:, 1 + tt * 512: 1 + (tt + 1) * 512], ps[:]),
                             reads=[psB], writes=[pjB[blk]])
                    else:
                        P.op("act", lambda e, ps=ps, blk=blk, tt=tt: e.activation(
                            gst[:, blk - 6, tt * 512:(tt + 1) * 512], ps[:], AF.Sigmoid), reads=[psB], writes=[gstB])
            c1 = slice(1, L + 1)
            cm = slice(0, L)
            cp = slice(2, L + 2)
            P.op("pool", lambda e: e.tensor_tensor(out=pj[1][:, c1], in0=pj[1][:, c1], in1=pj[2][:, c1], op=ALU.mult),
                 reads=[pjB[1], pjB[2]], writes=[pjB[1]])
            P.op("pool", lambda e, prm=prm: e.tensor_scalar(tmp[:], pj[1][:, cm], prm[:, 0:1], 0.0, op0=ALU.mult, op1=ALU.add),
                 reads=[pjB[1], prmB], writes=[tmpB])
            P.op("dve", lambda e, prm=prm: e.scalar_tensor_tensor(out=tmp[:], in0=pj[1][:, c1], scalar=prm[:, 1:2], in1=tmp[:],
                                                                  op0=ALU.mult, op1=ALU.add),
                 reads=[pjB[1], prmB, tmpB], writes=[tmpB])
            P.op("dve", lambda e, prm=prm: e.scalar_tensor_tensor(out=tmp[:], in0=pj[1][:, cp], scalar=prm[:, 2:3], in1=tmp[:],
                                                                  op0=ALU.mult, op1=ALU.add),
                 reads=[pjB[1], prmB, tmpB], writes=[tmpB])
            P.op("pool", lambda e: e.tensor_tensor(out=ya_st[:], in0=pj[0][:, c1], in1=tmp[:], op=ALU.mult),
                 reads=[pjB[0], tmpB], writes=[yaB])
            P.dma("pool", G["YA"][cc * 128:(cc + 1) * 128, b * L:(b + 1) * L], ya_st[:], reads=[yaB], writes=[dOut], key=yaB)

            def conv_h(src, srcB, k, dst, dstB):
                P.op("pool", lambda e, prm=prm: e.tensor_scalar(tmp[:] if dst is None else dst[:], src[:, cm],
                                                               prm[:, 3 + 3 * k:4 + 3 * k], prm[:, 12 + k:13 + k],
                                                               op0=ALU.mult, op1=ALU.add),
                     reads=[srcB, prmB], writes=[dstB])
            conv_h(pj[3], pjB[3], 0, tv, tvB)
            for j, sl in ((1, c1), (2, cp)):
                P.op("dve", lambda e, prm=prm, j=j, sl=sl: e.scalar_tensor_tensor(
                    out=tv[:], in0=pj[3][:, sl], scalar=prm[:, 3 + j:4 + j], in1=tv[:], op0=ALU.mult, op1=ALU.add),
                    reads=[pjB[3], prmB, tvB], writes=[tvB])
            conv_h(pj[4], pjB[4], 1, tmp, tmpB)
            for j, sl in ((1, c1), (2, cp)):
                P.op("dve", lambda e, prm=prm, j=j, sl=sl: e.scalar_tensor_tensor(
                    out=tmp[:], in0=pj[4][:, sl], scalar=prm[:, 6 + j:7 + j], in1=tmp[:], op0=ALU.mult, op1=ALU.add),
                    reads=[pjB[4], prmB, tmpB], writes=[tmpB])
            P.op("pool", lambda e: e.tensor_tensor(out=z_st[:], in0=tv[:], in1=tmp[:], op=ALU.mult),
                 reads=[tvB, tmpB], writes=[zB])
            conv_h(pj[5], pjB[5], 2, tmp, tmpB)
            P.op("dve", lambda e, prm=prm: e.scalar_tensor_tensor(
                out=tmp[:], in0=pj[5][:, c1], scalar=prm[:, 10:11], in1=tmp[:], op0=ALU.mult, op1=ALU.add),
                reads=[pjB[5], prmB, tmpB], writes=[tmpB])
            P.op("dve", lambda e, prm=prm: e.scalar_tensor_tensor(
                out=x0_st[:], in0=pj[5][:, cp], scalar=prm[:, 11:12], in1=tmp[:], op0=ALU.mult, op1=ALU.add),
                reads=[pjB[5], prmB, tmpB], writes=[x0B])
            P.dma("pool", G["X0T"][cc * 128:(cc + 1) * 128, b * L:(b + 1) * L], x0_st[:], reads=[x0B], writes=[dOut], key=x0B)
            for q in range(4):
                tp, tpB = tpring.next()

                def ft(e, tp=tp, q=q):
                    for j in range(8):
                        sc = q * 8 + j
                        ins = e.transpose(tp[:, j, :], z_st[:, sc * 128:(sc + 1) * 128], idb[:])
                    return ins
                P.op("pe", ft, reads=[zB, idbB], writes=[tpB])
                P.op("act", lambda e, tp=tp, q=q: e.copy(zt_st[:, q * 8:(q + 1) * 8, :], tp[:, :, :]), reads=[tpB], writes=[ztB])
            P.dma("pool", G["Z"][b, cc], zt_st[:].rearrange("p a c -> p (a c)"), reads=[ztB], writes=[dOut], key=ztB)
            for k in range(2):
                P.dma("pool", G["GT"][k * D + cc * 128: k * D + (cc + 1) * 128, b * L:(b + 1) * L], gst[:, k, :],
                      reads=[gstB], writes=[dOut], key=gstB)
    P.finish()


def phase_f(nc, G, l):
    P = Phase(nc, f"f{l}")
    posz, poszB = load_const(P, [33, L], F32, G["posz"])
    w1, w1B = load_const(P, [33, 64], F32, G["flt_w1"][l])
    w2, w2B = load_const(P, [64, 64], F32, G["flt_w2"][l])
    w3, w3B = load_const(P, [64, 2048], F32, G["flt_w3"][l])
    fp, fpB = load_const(P, [64, 4], F32, G["fprm"][l])
    tcol, tcolB = load_const(P, [128, 32], F32, G["tcol"])
    ndl, ndlB = load_const(P, [128, D], F32, G["negdelta"].partition_broadcast(128))
    hb_, hbB_ = load_const(P, [128, D], F32, G["hyena_bias"][l:l + 1, :].partition_broadcast(128))
    ones, onesB = load_const(P, [128, 128], F32, G["ones"])
    nyq, nyqB = load_const(P, [128, 32], BF16, G["nyqF"])
    eps6, eps6B = load_const(P, [128, 1], F32, G["eps6"])
    h1, h1B = P.sb([64, L], F32), Buf()
    h2, h2B = P.sb([64, L], F32), Buf()
    wr, wrB = P.sb([64, L], F32), Buf()
    psr = Ring(P, [128, 512], F32, 4, psum=True)
    ssq = [P.ps([128, 512], F32) for _ in range(4)]
    ssqB = [Buf() for _ in range(4)]
    dKR = Buf(multi=True)

    def sin_layer(src, srcB, w, wB, K, col, dst, dstB):
        for tt in range(8):
            ps, psB = psr.next()
            P.op("pe", lambda e, ps=ps, tt=tt: e.matmul(ps[0:64, :], w[0:K, :], src[0:K, tt * 512:(tt + 1) * 512],
                                                        start=True, stop=True), reads=[srcB, wB], writes=[psB])
            sl = slice(tt * 512, (tt + 1) * 512)
            P.op("dve", lambda e, ps=ps, sl=sl: e.tensor_scalar(dst[:, sl], ps[0:64, :], fp[:, col:col + 1], fp[:, col + 1:col + 2],
                                                                op0=ALU.add, op1=ALU.mult), reads=[psB, fpB], writes=[dstB])
        for _ in range(2):
            P.op("dve", lambda e: e.tensor_scalar(wr[:], dst[:], PI, -2 * PI, op0=ALU.is_gt, op1=ALU.mult), reads=[dstB], writes=[wrB])
            P.op("dve", lambda e: e.tensor_tensor(out=dst[:], in0=dst[:], in1=wr[:], op=ALU.add), reads=[dstB, wrB], writes=[dstB])
            P.op("dve", lambda e: e.tensor_scalar(wr[:], dst[:], -PI, 2 * PI, op0=ALU.is_lt, op1=ALU.mult), reads=[dstB], writes=[wrB])
            P.op("dve", lambda e: e.tensor_tensor(out=dst[:], in0=dst[:], in1=wr[:], op=ALU.add), reads=[dstB, wrB], writes=[dstB])
        P.op("dve", lambda e: e.tensor_scalar(dst[:], dst[:], 3.141592, -3.141592, op0=ALU.min, op1=ALU.max),
             reads=[dstB], writes=[dstB])
        P.op("act", lambda e: e.activation(dst[:], dst[:], AF.Sin), reads=[dstB], writes=[dstB])

    sin_layer(posz, poszB, w1, w1B, 33, 0, h1, h1B)
    sin_layer(h1, h1B, w2, w2B, 64, 2, h2, h2B)

    decr = Ring(P, [128, D], F32, 1)
    krr = Ring(P, [128, 512], F32, 2)
    sqr = Ring(P, [128, 512], F32, 2)
    for sc in range(32):
        dec, decB = decr.next()
        P.op("act", lambda e, dec=dec, sc=sc: e.activation(dec[:], ndl[:], AF.Exp, scale=tcol[:, sc:sc + 1]),
             reads=[ndlB, tcolB], writes=[decB])
        for q in range(4):
            ps, psB = psr.next()
            P.op("pe", lambda e, ps=ps, sc=sc, q=q: e.matmul(ps[:], h2[:, sc * 128:(sc + 1) * 128], w3[:, q * 512:(q + 1) * 512],
                                                             start=True, stop=True), reads=[h2B, w3B], writes=[psB])
            kr, krB = krr.next()
            ch0 = (q % 2) * 512
            P.op("dve", lambda e, ps=ps, kr=kr, dec=dec, ch0=ch0: e.tensor_tensor(out=kr[:], in0=ps[:], in1=dec[:, ch0:ch0 + 512],
                                                                                  op=ALU.mult), reads=[psB, decB], writes=[krB])
            P.dma("pool", G["KRAW"][sc * 128:(sc + 1) * 128, q * 512:(q + 1) * 512], kr[:], reads=[krB], writes=[dKR], key=krB)
            sq, sqB = sqr.next()
            P.op("act", lambda e, sq=sq, kr=kr: e.activation(sq[:], kr[:], AF.Square), reads=[krB], writes=[sqB])
            P.op("pe", lambda e, sq=sq, q=q, sc=sc: e.matmul(ssq[q][:], ones[:], sq[:], start=(sc == 0), stop=(sc == 31)),
                 reads=[sqB, onesB], writes=[ssqB[q]])
    rn, rnB = P.sb([128, 4, 512], F32), Buf()
    for q in range(4):
        P.op("act", lambda e, q=q: e.activation(rn[:, q, :], ssq[q][:], AF.Sqrt, bias=eps6[:, 0:1], scale=1.0),
             reads=[ssqB[q], eps6B], writes=[rnB])
    P.op("dve", lambda e: e.reciprocal(rn[:], rn[:]), reads=[rnB], writes=[rnB])

    A, AB = P.sb([128, 32, 512], BF16), Buf()
    Bm, BmB = P.sb([128, 32, 512], BF16), Buf()
    ldr = Ring(P, [128, 2, 512], F32, 2)
    fring = Ring(P, [128, 32, 128], BF16, 2)
    kor = Ring(P, [128, 512], F32, 2)
    dK = Buf(multi=True)
    for hh in range(2):
        for sc in range(32):
            ld, ldB = ldr.next()
            for d in range(2):
                q = d * 2 + hh
                P.dma("sp", ld[:, d, :], G["KRAW"][sc * 128:(sc + 1) * 128, q * 512:(q + 1) * 512], reads=[dKR], writes=[ldB], key=ldB)
            P.op("dve", lambda e, ld=ld, hh=hh: e.tensor_tensor(out=ld[:, 0, :], in0=ld[:, 0, :], in1=rn[:, hh, :], op=ALU.mult),
                 reads=[ldB, rnB], writes=[ldB])
            P.op("dve", lambda e, ld=ld, hh=hh: e.tensor_tensor(out=ld[:, 1, :], in0=ld[:, 1, :], in1=rn[:, 2 + hh, :], op=ALU.mult),
                 reads=[ldB, rnB], writes=[ldB])
            if sc == 0:
                P.op("dve", lambda e, ld=ld: e.memset(ld[0:1, 1, :], 0.0), reads=[ldB], writes=[ldB])
            P.op("pool", lambda e, ld=ld, sc=sc: e.tensor_tensor(out=A[:, sc, :], in0=ld[:, 0, :], in1=ld[:, 1, :], op=ALU.add),
                 reads=[ldB], writes=[AB])
            P.op("pool", lambda e, ld=ld, sc=sc: e.tensor_tensor(out=Bm[:, sc, :], in0=ld[:, 0, :], in1=ld[:, 1, :], op=ALU.subtract),
                 reads=[ldB], writes=[BmB])
        for fc in range(32):
            for kind in range(2):
                fb, fbB = fring.next()
                P.dma("sp", fb[:], G["dftF"][kind * 32 + fc], writes=[fbB], key=fbB)
                ps, psB = psr.next()
                src, srcB = (A, AB) if kind == 0 else (Bm, BmB)

                def mm(e, ps=ps, fb=fb, src=src):
                    for sc in range(32):
                        ins = e.matmul(ps[:], fb[:, sc, :], src[:, sc, :], start=(sc == 0), stop=(sc == 31))
                    return ins
                P.op("pe", mm, reads=[fbB, srcB], writes=[psB])
                ko, koB = kor.next()
                if kind == 0:
                    P.op("dve", lambda e, ko=ko, ps=ps, hh=hh: e.tensor_tensor(out=ko[:], in0=ps[:], in1=hb_[:, hh * 512:(hh + 1) * 512],
                                                                               op=ALU.add), reads=[psB, hbB_], writes=[koB])
                else:
                    P.op("act", lambda e, ko=ko, ps=ps: e.copy(ko[:], ps[:]), reads=[psB], writes=[koB])
                dst = G["KC"] if kind == 0 else G["KS"]
                P.dma("pool", dst[fc * 128:(fc + 1) * 128, hh * 512:(hh + 1) * 512], ko[:], reads=[koB], writes=[dK], key=koB)
        ps, psB = psr.next()

        def mmn(e, ps=ps):
            for sc in range(32):
                ins = e.matmul(ps[0:1, :], nyq[:, sc:sc + 1], A[:, sc, :], start=(sc == 0), stop=(sc == 31))
            return ins
        P.op("pe", mmn, reads=[nyqB, AB], writes=[psB])
        ko, koB = kor.next()
        P.op("dve", lambda e, ko=ko, ps=ps, hh=hh: e.tensor_tensor(out=ko[0:1, :], in0=ps[0:1, :], in1=hb_[0:1, hh * 512:(hh + 1) * 512],
                                                                   op=ALU.add), reads=[psB, hbB_], writes=[koB])
        P.dma("pool", G["KN"][0:1, hh * 512:(hh + 1) * 512], ko[0:1, :], reads=[koB], writes=[dK], key=koB)
    P.finish()


def phase_h(nc, G, l):
    P = Phase(nc, f"h{l}")
    nyqF, nyqFB = load_const(P, [128, 32], BF16, G["nyqF"])
    nyqG, nyqGB = load_const(P, [1, L], BF16, G["nyqG"])
    z_sb, zB = P.sb([128, 4, 32, 128], BF16), Buf()
    fring = Ring(P, [128, 32, 128], BF16, 3)
    kring = Ring(P, [128, 2, 512], F32, 2)
    tring = Ring(P, [128, 512], F32, 8)
    Y, YB = P.sb([128, 2, 32, 512], BF16), Buf()
    Yn, YnB = P.sb([1, 512], BF16), Buf()
    knt, kntB = P.sb([1, 512], F32), Buf()
    gring = Ring(P, [128, 16, 512], BF16, 2)
    x0r = Ring(P, [128, 512], BF16, 3)
    outr = Ring(P, [128, 512], BF16, 3)
    psz = Ring(P, [128, 512], F32, 4, psum=True)
    acc = [P.ps([128, 512], F32) for _ in range(4)]
    accB = [Buf() for _ in range(4)]
    dOut = Buf(multi=True)
    for b in range(NBC):
        for g in range(2):
            for j in range(4):
                P.dma("sp", z_sb[:, j, :, :].rearrange("p a c -> p (a c)"), G["Z"][b, g * 4 + j], writes=[zB], key=zB)
            P.dma("sp", knt[:], G["KN"][0:1, g * 512:(g + 1) * 512], writes=[kntB], key=kntB)
            for fc in range(32):
                kt, ktB = kring.next()
                P.dma("sp", kt[:, 0, :], G["KC"][fc * 128:(fc + 1) * 128, g * 512:(g + 1) * 512], writes=[ktB], key=ktB)
                P.dma("sp", kt[:, 1, :], G["KS"][fc * 128:(fc + 1) * 128, g * 512:(g + 1) * 512], writes=[ktB], key=ktB)
                zp = []
                for kind in range(2):
                    fb, fbB = fring.next()
                    P.dma("sp", fb[:], G["dftF"][kind * 32 + fc], writes=[fbB], key=fbB)
                    ps, psB = psz.next()

                    def mm(e, ps=ps, fb=fb):
                        for sc in range(32):
                            ins = e.matmul(ps[:], fb[:, sc, :], z_sb[:, :, sc, :], start=(sc == 0), stop=(sc == 31))
                        return ins
                    P.op("pe", mm, reads=[fbB, zB], writes=[psB])
                    zp.append((ps, psB))
                (zc, zcB), (zs, zsB) = zp
                t = [tring.next() for _ in range(4)]
                P.op("dve", lambda e, zc=zc, kt=kt, t=t: e.tensor_tensor(out=t[0][0][:], in0=zc[:], in1=kt[:, 0, :], op=ALU.mult),
                     reads=[zcB, ktB], writes=[t[0][1]])
                P.op("dve", lambda e, zs=zs, kt=kt, t=t: e.tensor_tensor(out=t[1][0][:], in0=zs[:], in1=kt[:, 1, :], op=ALU.mult),
                     reads=[zsB, ktB], writes=[t[1][1]])
                P.op("dve", lambda e, zc=zc, kt=kt, t=t: e.tensor_tensor(out=t[2][0][:], in0=zc[:], in1=kt[:, 1, :], op=ALU.mult),
                     reads=[zcB, ktB], writes=[t[2][1]])
                P.op("dve", lambda e, zs=zs, kt=kt, t=t: e.tensor_tensor(out=t[3][0][:], in0=zs[:], in1=kt[:, 0, :], op=ALU.mult),
                     reads=[zsB, ktB], writes=[t[3][1]])
                P.op("pool", lambda e, t=t, fc=fc: e.tensor_tensor(out=Y[:, 0, fc, :], in0=t[0][0][:], in1=t[1][0][:], op=ALU.subtract),
                     reads=[t[0][1], t[1][1]], writes=[YB])
                P.op("pool", lambda e, t=t, fc=fc: e.tensor_tensor(out=Y[:, 1, fc, :], in0=t[2][0][:], in1=t[3][0][:], op=ALU.add),
                     reads=[t[2][1], t[3][1]], writes=[YB])
            ps, psB = psz.next()

            def mmn(e, ps=ps):
                for sc in range(32):
                    ins = e.matmul(ps[0:1, :], nyqF[:, sc:sc + 1], z_sb[:, :, sc, :], start=(sc == 0), stop=(sc == 31))
                return ins
            P.op("pe", mmn, reads=[nyqFB, zB], writes=[psB])
            P.op("dve", lambda e, ps=ps: e.tensor_tensor(out=Yn[0:1, :], in0=ps[0:1, :], in1=knt[0:1, :], op=ALU.mult),
                 reads=[psB, kntB], writes=[YnB])
            for tt in range(8):
                for piece in range(4):
                    gp, gpB = gring.next()
                    P.dma("sp", gp[:], G["dftG"][tt, piece], writes=[gpB], key=gpB)
                    kind = piece // 2
                    for j in range(4):
                        def mmi(e, gp=gp, j=j, piece=piece, kind=kind, tt=tt):
                            for i in range(16):
                                fc = (piece % 2) * 16 + i
                                ins = e.matmul(acc[j][:], Y[:, kind, fc, j * 128:(j + 1) * 128], gp[:, i, :],
                                               start=(piece == 0 and i == 0), stop=False)
                            if piece == 3:
                                ins = e.matmul(acc[j][:], Yn[0:1, j * 128:(j + 1) * 128], nyqG[0:1, tt * 512:(tt + 1) * 512],
                                               start=False, stop=True)
                            return ins
                        rd = [gpB, YB] + ([YnB, nyqGB] if piece == 3 else [])
                        P.op("pe", mmi, reads=rd, writes=[accB[j]])
                for j in range(4):
                    ch0 = (g * 4 + j) * 128
                    x0t, x0B = x0r.next()
                    P.dma("sp", x0t[:], G["X0T"][ch0:ch0 + 128, b * L + tt * 512: b * L + (tt + 1) * 512], writes=[x0B], key=x0B)
                    ot, otB = outr.next()
                    P.op("dve", lambda e, ot=ot, j=j, x0t=x0t: e.tensor_tensor(out=ot[:], in0=acc[j][:], in1=x0t[:], op=ALU.mult),
                         reads=[accB[j], x0B], writes=[otB])
                    P.dma("pool", G["YH"][ch0:ch0 + 128, b * L + tt * 512: b * L + (tt + 1) * 512], ot[:], reads=[otB],
                          writes=[dOut], key=otB)
    P.finish()


def load_w_cast(P, dst, dstB, src_ap, nk, ncols, col0=0):
    v = src_ap.rearrange("(kc p) n -> p kc n", p=128)
    for kc in range(nk):
        for c in range(0, ncols, 2048):
            w = min(2048, ncols - c)
            P.dma("pool", dst[:, kc, c:c + w], v[:, kc, col0 + c:col0 + c + w], writes=[dstB], key=dstB)


def phase_o(nc, G, l, want_logits):
    P = Phase(nc, f"o{l}")
    C = LNCtx(P, G, G["ln_mix_g"][l:l + 1, :], G["ln_mix_b"][l:l + 1, :])
    wa, waB = P.sb([128, 8, D], BF16), Buf()
    wh, whB = P.sb([128, 8, D], BF16), Buf()
    wo, woB = P.sb([128, 8, D], BF16), Buf()
    load_w_cast(P, wa, waB, G["w_a_out"][l], 8, D)
    load_w_cast(P, wh, whB, G["w_h_out"][l], 8, D)
    load_w_cast(P, wo, woB, G["w_o"][l], 8, D)
    yar = Ring(P, [128, 8, 512], BF16, 2)
    yhr = Ring(P, [128, 8, 512], BF16, 2)
    gtr = Ring(P, [128, 16, 512], BF16, 2)
    mgr = Ring(P, [128, 8, 512], BF16, 2)
    t1r = Ring(P, [128, 512], F32, 2)
    t2r = Ring(P, [128, 512], F32, 2)
    hin = Ring(P, [128, D], F32, 3)
    rr = Ring(P, [128, D], F32, 2)
    hnr = Ring(P, [128, D], F32, 2)
    psp = Ring(P, [128, 512], F32, 4, psum=True)
    pso = Ring(P, [128, 512], F32, 2, psum=True)
    YAv = G["YA"].rearrange("(kc p) t -> p kc t", p=128)
    YHv = G["YH"].rearrange("(kc p) t -> p kc t", p=128)
    GTv = G["GT"].rearrange("(kc p) t -> p kc t", p=128)
    HTv = ht_view(G)
    dH, dHT = Buf(multi=True), Buf(multi=True)
    if want_logits:
        idf, idfB = load_const(P, [128, 128], F32, G["identf"])
        rt, rtB = load_const(P, [128, 8, NE], F32, G["moe_router"][0].rearrange("(kc p) e -> p kc e", p=128))
        hTf = Ring(P, [128, 8, 128], F32, 2)
        lgr = Ring(P, [128, NE], F32, 2)
        dLG = Buf(multi=True)
    for tt in range(T // 512):
        ts = slice(tt * 512, (tt + 1) * 512)
        ya, yaB = yar.next()
        yh, yhB = yhr.next()
        gt, gtB = gtr.next()
        P.dma("sp", ya[:], YAv[:, :, ts], writes=[yaB], key=yaB)
        P.dma("sp", yh[:], YHv[:, :, ts], writes=[yhB], key=yhB)
        P.dma("sp", gt[:], GTv[:, :, ts], writes=[gtB], key=gtB)
        mg, mgB = mgr.next()
        for j in range(8):
            pa, paB = psp.next()
            ph, phB = psp.next()

            def mma(e, pa=pa, ya=ya, j=j):
                for kc in range(8):
                    ins = e.matmul(pa[:], wa[:, kc, j * 128:(j + 1) * 128], ya[:, kc, :], start=(kc == 0), stop=(kc == 7))
                return ins

            def mmh(e, ph=ph, yh=yh, j=j):
                for kc in range(8):
                    ins = e.matmul(ph[:], wh[:, kc, j * 128:(j + 1) * 128], yh[:, kc, :], start=(kc == 0), stop=(kc == 7))
                return ins
            P.op("pe", mma, reads=[waB, yaB], writes=[paB])
            P.op("pe", mmh, reads=[whB, yhB], writes=[phB])
            t1, t1B = t1r.next()
            t2, t2B = t2r.next()
            P.op("dve", lambda e, t1=t1, pa=pa, gt=gt, j=j: e.tensor_tensor(out=t1[:], in0=pa[:], in1=gt[:, j, :], op=ALU.mult),
                 reads=[paB, gtB], writes=[t1B])
            P.op("dve", lambda e, t2=t2, ph=ph, gt=gt, j=j: e.tensor_tensor(out=t2[:], in0=ph[:], in1=gt[:, 8 + j, :], op=ALU.mult),
                 reads=[phB, gtB], writes=[t2B])
            P.op("pool", lambda e, t1=t1, t2=t2, mg=mg, j=j: e.tensor_tensor(out=mg[:, j, :], in0=t1[:], in1=t2[:], op=ALU.add),
                 reads=[t1B, t2B], writes=[mgB])
        stage, stageB = C.stage.next()
        for i in range(4):
            t0 = tt * 512 + i * 128
            hi, hiB = hin.next()
            P.dma("sp", hi[:], G["Hsrc"][t0:t0 + 128, :], writes=[hiB], key=hiB)
            r, rB = rr.next()
            for hf in range(2):
                po, poB = pso.next()

                def mmo(e, po=po, mg=mg, i=i, hf=hf):
                    for kc in range(8):
                        ins = e.matmul(po[:], mg[:, kc, i * 128:(i + 1) * 128], wo[:, kc, hf * 512:(hf + 1) * 512],
                                       start=(kc == 0), stop=(kc == 7))
                    return ins
                P.op("pe", mmo, reads=[mgB, woB], writes=[poB])
                P.op("dve", lambda e, r=r, hi=hi, po=po, hf=hf: e.scalar_tensor_tensor(
                    out=r[:, hf * 512:(hf + 1) * 512], in0=hi[:, hf * 512:(hf + 1) * 512], scalar=float(ALPHA), in1=po[:],
                    op0=ALU.mult, op1=ALU.add), reads=[hiB, poB], writes=[rB])
            hn, hnB = hnr.next()
            C.norm(r, rB, hn, hnB)
            P.dma("pool", G["H"][t0:t0 + 128, :], hn[:], reads=[hnB], writes=[dH], key=hnB)
            C.transpose_into(hn, hnB, stage, stageB, i)
            if want_logits:
                hT, hTB = hTf.next()
                for q in range(2):
                    pt, ptB = pso.next()

                    def ftf(e, pt=pt, hn=hn, q=q):
                        for jj in range(4):
                            j = q * 4 + jj
                            ins = e.transpose(pt[:, jj * 128:(jj + 1) * 128], hn[:, j * 128:(j + 1) * 128], idf[:])
                        return ins
                    P.op("pe", ftf, reads=[hnB, idfB], writes=[ptB])
                    P.op("act", lambda e, pt=pt, hT=hT, q=q: e.copy(hT[:, q * 4:(q + 1) * 4, :],
                                                                   pt[:].rearrange("p (a c) -> p a c", c=128)),
                         reads=[ptB], writes=[hTB])
                pl, plB = pso.next()

                def mml(e, pl=pl, hT=hT):
                    for kc in range(8):
                        ins = e.matmul(pl[:, 0:NE], hT[:, kc, :], rt[:, kc, :], start=(kc == 0), stop=(kc == 7))
                    return ins
                P.op("pe", mml, reads=[hTB, rtB], writes=[plB])
                lg, lgB = lgr.next()
                P.op("act", lambda e, lg=lg, pl=pl: e.copy(lg[:], pl[:, 0:NE]), reads=[plB], writes=[lgB])
                P.dma("pool", G["LG"][t0:t0 + 128, :], lg[:], reads=[lgB], writes=[dLG], key=lgB)
        P.dma("pool", HTv[:, :, ts], stage[:], reads=[stageB], writes=[dHT], key=stageB)
    P.finish()


def phase_ffn(nc, G, l, moe):
    P = Phase(nc, f"ffn{l}")
    TT = 1024 if moe else 512
    NH = TT // 512
    NS = TT // 128
    nff = (DFE if moe else DFF) // 128
    C = LNCtx(P, G, G["ln_ffn_g"][l:l + 1, :], G["ln_ffn_b"][l:l + 1, :], want_T=not moe)
    HTv = ht_view(G)
    HTs = hts_view(G)
    htr = Ring(P, [128, 8, TT], BF16, 2)
    aT, aTB = P.sb([128, nff, TT], BF16), Buf()
    w13r = Ring(P, [128, 2, 8, 128], BF16, 3)
    w2c = Ring(P, [128, 512], BF16, 4)
    sg = Ring(P, [128, 512], F32, 3)
    psg = Ring(P, [128, 512], F32, 8 if moe else 4, psum=True)
    hin = Ring(P, [128, D], F32, 3)
    accr = Ring(P, [128, D], F32, NS + 1)
    hnr = Ring(P, [128, D], F32, 2)
    dH, dHT = Buf(multi=True), Buf(multi=True)
    if moe:
        lgr = Ring(P, [128, NE], F32, NS + 1)
        cwr = Ring(P, [128, NE], F32, NS + 1)
        m8r = Ring(P, [128, 8], F32, 2)
        scr = Ring(P, [128, 3 * NE], F32, 2)
    nexp = NE if moe else 1
    for tt in range(T // TT):
        ts = slice(tt * TT, (tt + 1) * TT)
        ht, htB = htr.next()
        P.dma("sp", ht[:], HTs[:, :, ts], writes=[htB], key=htB)
        accs = []
        cws = []
        for i in range(TT // 128):
            t0 = tt * TT + i * 128
            hi, hiB = hin.next()
            P.dma("sp", hi[:], G["Hsrc"][t0:t0 + 128, :], writes=[hiB], key=hiB)
            ac, acB = accr.next()
            P.op("pool", lambda e, ac=ac, hi=hi: e.tensor_scalar(ac[:], hi[:], float(ALPHA), 0.0, op0=ALU.mult, op1=ALU.add),
                 reads=[hiB], writes=[acB])
            accs.append((ac, acB))
            if moe:
                lg, lgB = lgr.next()
                P.dma("sp", lg[:], G["LGsrc"][t0:t0 + 128, :], writes=[lgB], key=lgB)
                cw, cwB = cwr.next()
                m8, m8B = m8r.next()
                sc_, scB = scr.next()
                P.op("dve", lambda e, m8=m8, lg=lg: e.max(m8[:], lg[:]), reads=[lgB], writes=[m8B])
                P.op("dve", lambda e, sc_=sc_, lg=lg, m8=m8: e.tensor_scalar(sc_[:, 0:NE], lg[:], m8[:, 1:2], None, op0=ALU.is_ge),
                     reads=[lgB, m8B], writes=[scB])
                P.op("dve", lambda e, sc_=sc_, m8=m8: e.tensor_scalar(sc_[:, 2 * NE:2 * NE + 1], m8[:, 0:1], -1.0, None, op0=ALU.mult),
                     reads=[m8B, scB], writes=[scB])
                P.op("act", lambda e, sc_=sc_, lg=lg: e.activation(sc_[:, NE:2 * NE], lg[:], AF.Exp, bias=sc_[:, 2 * NE:2 * NE + 1], scale=1.0),
                     reads=[lgB, scB], writes=[scB])
                P.op("dve", lambda e, sc_=sc_: e.tensor_tensor(out=sc_[:, NE:2 * NE], in0=sc_[:, NE:2 * NE], in1=sc_[:, 0:NE], op=ALU.mult),
                     reads=[scB], writes=[scB])
                P.op("dve", lambda e, sc_=sc_: e.tensor_reduce(out=sc_[:, 2 * NE + 1:2 * NE + 2], in_=sc_[:, NE:2 * NE],
                                                               axis=mybir.AxisListType.X, op=ALU.add), reads=[scB], writes=[scB])
                P.op("dve", lambda e, sc_=sc_: e.reciprocal(sc_[:, 2 * NE + 1:2 * NE + 2], sc_[:, 2 * NE + 1:2 * NE + 2]),
                     reads=[scB], writes=[scB])
                P.op("dve", lambda e, sc_=sc_, cw=cw: e.tensor_scalar(cw[:], sc_[:, NE:2 * NE], sc_[:, 2 * NE + 1:2 * NE + 2], None, op0=ALU.mult),
                     reads=[scB], writes=[cwB])
                cws.append((cw, cwB))
        for ex in range(nexp):
            if moe:
                W1, W3, W2 = G["moe_w1"][0, ex], G["moe_w3"][0, ex], G["moe_w2"][0, ex]
            else:
                W1, W3, W2 = G["ffn_w1"][0], G["ffn_w3"][0], G["ffn_w2"][0]
            w1v = W1.rearrange("(kc p) n -> p kc n", p=128)
            w3v = W3.rearrange("(kc p) n -> p kc n", p=128)
            for c in range(nff):
                w13, w13B = w13r.next()
                P.dma("pool", w13[:, 0, :, :], w1v[:, :, c * 128:(c + 1) * 128], writes=[w13B], key=w13B)
                P.dma("pool", w13[:, 1, :, :], w3v[:, :, c * 128:(c + 1) * 128], writes=[w13B], key=w13B)
                for hh in range(NH):
                    pg, pgB = psg.next()
                    pu, puB = psg.next()
                    hsl = slice(hh * 512, (hh + 1) * 512)

                    def mmg(e, pg=pg, w13=w13, ht=ht, which=0, hsl=hsl):
                        for kc in range(8):
                            ins = e.matmul(pg[:], w13[:, which, kc, :], ht[:, kc, hsl], start=(kc == 0), stop=(kc == 7))
                        return ins
                    P.op("pe", mmg, reads=[w13B, htB], writes=[pgB])
                    P.op("pe", lambda e, pu=pu, w13=w13, ht=ht, hsl=hsl, mmg=mmg: mmg(e, pu, w13, ht, 1, hsl), reads=[w13B, htB], writes=[puB])
                    s_, sB = sg.next()
                    P.op("act", lambda e, s_=s_, pg=pg: e.activation(s_[:], pg[:], AF.Silu), reads=[pgB], writes=[sB])
                    P.op("dve", lambda e, s_=s_, pu=pu, c=c, hsl=hsl: e.tensor_tensor(out=aT[:, c, hsl], in0=pu[:], in1=s_[:], op=ALU.mult),
                         reads=[puB, sB], writes=[aTB])
            for hf in range(2):
                pyl = [psg.next() for _ in range(TT // 128)]
                for c in range(nff):
                    w2t, w2B = w2c.next()
                    P.dma("pool", w2t[:, 0:512], W2[c * 128:(c + 1) * 128, hf * 512:(hf + 1) * 512], writes=[w2B], key=w2B)
                    for i in range(TT // 128):
                        py, pyB = pyl[i]
                        P.op("pe", lambda e, py=py, i=i, c=c, w2t=w2t: e.matmul(py[:], aT[:, c, i * 128:(i + 1) * 128], w2t[:, 0:512],
                                                                               start=(c == 0), stop=(c == nff - 1)),
                             reads=[aTB, w2B], writes=[pyB])
                for i in range(TT // 128):
                    py, pyB = pyl[i]
                    ac, acB = accs[i]
                    hs = slice(hf * 512, (hf + 1) * 512)
                    if moe:
                        cw, cwB = cws[i]
                        P.op("dve", lambda e, ac=ac, py=py, cw=cw, ex=ex, hs=hs: e.scalar_tensor_tensor(
                            out=ac[:, hs], in0=py[:], scalar=cw[:, ex:ex + 1], in1=ac[:, hs], op0=ALU.mult, op1=ALU.add),
                            reads=[pyB, cwB, acB], writes=[acB])
                    else:
                        P.op("dve", lambda e, ac=ac, py=py, hs=hs: e.tensor_tensor(out=ac[:, hs], in0=py[:], in1=ac[:, hs], op=ALU.add),
                             reads=[pyB, acB], writes=[acB])
        if not moe:
            stage, stageB = C.stage.next()
        for i in range(TT // 128):
            t0 = tt * TT + i * 128
            ac, acB = accs[i]
            hn, hnB = hnr.next()
            C.norm(ac, acB, hn, hnB)
            if moe:
                P.dma("act", G["out"][t0:t0 + 128, :], hn[:], reads=[hnB], writes=[dH], key=hnB)
            else:
                P.dma("act", G["H"][t0:t0 + 128, :], hn[:], reads=[hnB], writes=[dH], key=hnB)
                C.transpose_into(hn, hnB, stage, stageB, i)
        if not moe:
            P.dma("act", HTv[:, :, ts], stage[:], reads=[stageB], writes=[dHT], key=stageB)
    P.finish()


def phase_r(nc, G):
    P = Phase(nc, "r1")
    NTI = T // 128
    tri, triB = load_const(P, [128, 128], BF16, G["triu"])
    onb, onbB = load_const(P, [128, 128], BF16, G["onesb"])
    lg_all, lgB = load_const(P, [128, NTI, NE], F32, G["LGsrc"].rearrange("(i p) e -> p i e", p=128))
    zt, ztB = P.sb([128, 8, D], BF16), Buf()
    P.op("pool", lambda e: e.memset(zt[:], 0.0), writes=[ztB])
    zfill = Buf()
    XSz = G["XS"].rearrange("(a p) d -> p a d", p=128)
    for a in range(NSLOT // 1024):
        P.dma("act", XSz[:, a * 8:(a + 1) * 8, :], zt[:], reads=[ztB], writes=[zfill], key=zfill)
    m8_all, m8B = P.sb([128, NTI, 8], F32), Buf()
    mask_all, maskB = P.sb([128, NTI, NE], F32), Buf()
    ex_all, exB = P.sb([128, NTI, NE], F32), Buf()
    cw_all, cwB = P.sb([128, NTI, NE], F32), Buf()
    den, denB = P.sb([128, NTI], F32), Buf()
    maskb, maskbB = P.sb([128, NTI * NE], BF16), Buf()
    tot, totB = P.sb([128, NTI, NE], F32), Buf()
    wit, witB = P.sb([128, NTI, NE], F32), Buf()
    base, baseB = P.sb([128, NTI, NE], F32), Buf()
    pw, pwB = P.ps([128, 512], F32), Buf()
    pt, ptB = P.ps([128, 512], F32), Buf()
    sm, smB = P.sb([128, 64], F32), Buf()
    etf, etfB = P.sb([128, NT], F32), Buf()
    eti, etiB = P.sb([128, NT], I32), Buf()
    slf, slfB = P.sb([128, NTI, 2], F32), Buf()
    sli, sliB = P.sb([128, NTI * 2], I32), Buf()
    cws, cwsB = P.sb([128, NTI, 2], F32), Buf()
    vr = Ring(P, [128, 32], F32, 2)
    flat = lambda t: t[:].rearrange("p a c -> p (a c)")
    for i in range(NTI):
        P.op("dve", lambda e, i=i: e.max(m8_all[:, i, :], lg_all[:, i, :]), reads=[lgB], writes=[m8B])
        P.op("dve", lambda e, i=i: e.tensor_scalar(mask_all[:, i, :], lg_all[:, i, :], m8_all[:, i, 1:2], None, op0=ALU.is_ge),
             reads=[lgB, m8B], writes=[maskB])
        P.op("dve", lambda e, i=i: e.tensor_scalar(ex_all[:, i, :], lg_all[:, i, :], m8_all[:, i, 0:1], None, op0=ALU.subtract),
             reads=[lgB, m8B], writes=[exB])
    P.op("act", lambda e: e.activation(flat(ex_all), flat(ex_all), AF.Exp), reads=[exB], writes=[exB])
    P.op("dve", lambda e: e.tensor_tensor(out=flat(ex_all), in0=flat(ex_all), in1=flat(mask_all), op=ALU.mult),
         reads=[exB, maskB], writes=[exB])
    for i in range(NTI):
        P.op("dve", lambda e, i=i: e.tensor_reduce(out=den[:, i:i + 1], in_=ex_all[:, i, :], axis=mybir.AxisListType.X, op=ALU.add),
             reads=[exB], writes=[denB])
    P.op("dve", lambda e: e.reciprocal(den[:], den[:]), reads=[denB], writes=[denB])
    for i in range(NTI):
        P.op("dve", lambda e, i=i: e.tensor_scalar(cw_all[:, i, :], ex_all[:, i, :], den[:, i:i + 1], None, op0=ALU.mult),
             reads=[exB, denB], writes=[cwB])
    P.op("dve", lambda e: e.tensor_copy(maskb[:], flat(mask_all)), reads=[maskB], writes=[maskbB])
    P.op("pe", lambda e: e.matmul(pw[:], tri[:], maskb[:], start=True, stop=True), reads=[triB, maskbB], writes=[pwB])
    P.op("pe", lambda e: e.matmul(pt[:], onb[:], maskb[:], start=True, stop=True), reads=[onbB, maskbB], writes=[ptB])
    P.op("dve", lambda e: e.tensor_copy(flat(wit), pw[:]), reads=[pwB], writes=[witB])
    P.op("act", lambda e: e.copy(flat(tot), pt[:]), reads=[ptB], writes=[totB])
    P.op("dve", lambda e: e.memset(base[:, 0, :], 0.0), writes=[baseB])
    for i in range(1, NTI):
        P.op("dve", lambda e, i=i: e.tensor_tensor(out=base[:, i, :], in0=base[:, i - 1, :], in1=tot[:, i - 1, :], op=ALU.add),
             reads=[baseB, totB], writes=[baseB])
    P.op("dve", lambda e: e.tensor_tensor(out=sm[:, 0:8], in0=base[:, NTI - 1, :], in1=tot[:, NTI - 1, :], op=ALU.add),
         reads=[baseB, totB], writes=[smB])
    P.op("dve", lambda e: e.tensor_scalar(sm[:, 8:16], sm[:, 0:8], 0.0, None, op0=ALU.is_gt), reads=[smB], writes=[smB])
    for j in range(1, 8):
        P.op("dve", lambda e, j=j: e.scalar_tensor_tensor(out=sm[:, 8:16], in0=sm[:, 0:8], scalar=float(j * 1024), in1=sm[:, 8:16],
                                                          op0=ALU.is_gt, op1=ALU.add), reads=[smB], writes=[smB])
    P.op("dve", lambda e: e.tensor_copy(sm[:, 16:17], sm[:, 8:9]), reads=[smB], writes=[smB])
    for x in range(1, 8):
        P.op("dve", lambda e, x=x: e.tensor_tensor(out=sm[:, 16 + x:17 + x], in0=sm[:, 15 + x:16 + x], in1=sm[:, 8 + x:9 + x], op=ALU.add),
             reads=[smB], writes=[smB])
    P.op("dve", lambda e: e.tensor_tensor(out=sm[:, 24:32], in0=sm[:, 16:24], in1=sm[:, 8:16], op=ALU.subtract), reads=[smB], writes=[smB])
    P.op("dve", lambda e: e.tensor_scalar(sm[:, 24:32], sm[:, 24:32], 1024.0, None, op0=ALU.mult), reads=[smB], writes=[smB])
    for j in range(NT):
        P.op("dve", lambda e, j=j: e.tensor_scalar(sm[:, 32:40], sm[:, 16:24], float(j), None, op0=ALU.is_le), reads=[smB], writes=[smB])
        P.op("dve", lambda e, j=j: e.tensor_reduce(out=etf[:, j:j + 1], in_=sm[:, 32:40], axis=mybir.AxisListType.X, op=ALU.add),
             reads=[smB], writes=[etfB])
    P.op("dve", lambda e: e.tensor_scalar(etf[:], etf[:], 7.0, None, op0=ALU.min), reads=[etfB], writes=[etfB])
    P.op("dve", lambda e: e.tensor_copy(eti[:], etf[:]), reads=[etfB], writes=[etiB])
    dS = Buf(multi=True)
    P.dma("sp", G["ETILE"][0:1, :], eti[0:1, :], reads=[etiB], writes=[dS], key=etiB)
    cp_, cpB = load_const(P, [128, NFC], F32, G["cidx"])
    wif, wifB = P.sb([128, NT, NFC], F32), Buf()
    wii, wiiB = P.sb([128, NT * NFC], I32), Buf()
    P.op("dve", lambda e: e.tensor_scalar(etf[:], etf[:], float(DFE), None, op0=ALU.mult), reads=[etfB, etiB], writes=[etfB])
    for j in range(NT):
        P.op("dve", lambda e, j=j: e.tensor_scalar(wif[:, j, :], cp_[:], etf[:, j:j + 1], None, op0=ALU.add),
             reads=[cpB, etfB], writes=[wifB])
    P.op("dve", lambda e: e.tensor_copy(wii[:], wif[:].rearrange("p a c -> p (a c)")), reads=[wifB], writes=[wiiB])
    P.dma("sp", G["WIDX"], wii[:], reads=[wiiB], writes=[dS], key=wiiB)
    for i in range(NTI):
        v, vB = vr.next()
        P.op("dve", lambda e, v=v, i=i: e.tensor_tensor(out=v[:, 0:8], in0=wit[:, i, :], in1=base[:, i, :], op=ALU.add),
             reads=[witB, baseB], writes=[vB])
        P.op("dve", lambda e, v=v: e.tensor_tensor(out=v[:, 0:8], in0=v[:, 0:8], in1=sm[:, 24:32], op=ALU.add), reads=[vB, smB], writes=[vB])
        P.op("dve", lambda e, v=v, i=i: e.scalar_tensor_tensor(out=v[:, 0:8], in0=v[:, 0:8], scalar=1.0, in1=mask_all[:, i, :],
                                                               op0=ALU.add, op1=ALU.mult), reads=[vB, maskB], writes=[vB])
        P.op("dve", lambda e, v=v: e.tensor_scalar(v[:, 0:8], v[:, 0:8], -1.0, None, op0=ALU.add), reads=[vB], writes=[vB])
        P.op("dve", lambda e, v=v: e.max(v[:, 8:16], v[:, 0:8]), reads=[vB], writes=[vB])
        P.op("dve", lambda e, v=v, i=i: e.tensor_copy(slf[:, i, :], v[:, 8:10]), reads=[vB], writes=[slfB])
        for k in range(2):
            P.op("dve", lambda e, v=v, i=i, k=k: e.scalar_tensor_tensor(out=v[:, 16:24], in0=v[:, 0:8], scalar=v[:, 8 + k:9 + k],
                                                                        in1=cw_all[:, i, :], op0=ALU.is_equal, op1=ALU.mult),
                 reads=[vB, cwB], writes=[vB])
            P.op("dve", lambda e, v=v, i=i, k=k: e.tensor_reduce(out=cws[:, i, k:k + 1], in_=v[:, 16:24], axis=mybir.AxisListType.X, op=ALU.add),
                 reads=[vB], writes=[cwsB])
    P.op("dve", lambda e: e.tensor_copy(sli[:], flat(slf)), reads=[slfB], writes=[sliB])
    P.dma("sp", G["SLOT"].rearrange("(i p) k -> p i k", p=128), sli[:].rearrange("p (i k) -> p i k", k=2), reads=[sliB], writes=[dS], key=sliB)
    P.dma("sp", G["CWS"].rearrange("(i p) k -> p i k", p=128), cws[:], reads=[cwsB], writes=[dS], key=cwsB)
    sl2, sl2B = P.sb([128, NTI * 2], I32), Buf()
    P.dma("sp", sl2[:].rearrange("p (i k) -> p i k", k=2), G["SLOT"].rearrange("(i p) k -> p i k", p=128), reads=[dS], writes=[sl2B], key=sl2B)
    hin = Ring(P, [128, D], F32, 3)
    hbr = Ring(P, [128, D], BF16, 3)
    dXS = Buf(multi=True)
    XS = G["XS"]
    for i in range(NTI):
        hi, hiB = hin.next()
        P.dma("sp", hi[:], G["Hsrc"][i * 128:(i + 1) * 128, :], writes=[hiB], key=hiB)
        hb, hbB = hbr.next()
        P.op("act", lambda e, hb=hb, hi=hi: e.copy(hb[:], hi[:]), reads=[hiB], writes=[hbB])
        for k in range(2):
            P.dma("pool", None, None, reads=[hbB, sl2B, zfill], writes=[dXS], key=hbB,
                  fn=lambda e, hb=hb, i=i, k=k: e.indirect_dma_start(
                      out=XS[:, :], out_offset=bass.IndirectOffsetOnAxis(ap=sl2[:, 2 * i + k:2 * i + k + 1], axis=0),
                      in_=hb[:, :], in_offset=None, bounds_check=P.bcreg(e), oob_is_err=False))
    P.finish()


def phase_x(nc, G):
    P = Phase(nc, "x1")
    idb, idbB = load_const(P, [128, 128], BF16, G["identb"])
    xr = Ring(P, [128, D], BF16, 4)
    tpr = Ring(P, [128, 8, 128], BF16, 4, psum=True)
    stg = Ring(P, [128, 8, 512], BF16, 3)
    XSTv = G["XST"].rearrange("(kc p) t -> p kc t", p=128)
    dX = Buf(multi=True)
    n = 0
    for g in range(NSLOT // 512):
        stage, stageB = stg.next()
        for i in range(4):
            s0 = g * 512 + i * 128
            xs, xsB = xr.next()
            P.dma("sp", xs[:], G["XS"][s0:s0 + 128, :], writes=[xsB], key=xsB)
            tp, tpB = tpr.next()

            def ft(e, tp=tp, xs=xs):
                for j in range(8):
                    ins = e.transpose(tp[:, j, :], xs[:, j * 128:(j + 1) * 128], idb[:])
                return ins
            P.op("pe", ft, reads=[xsB, idbB], writes=[tpB])
            if n % 2 == 0:
                P.op("dve", lambda e, tp=tp, stage=stage, i=i: e.tensor_copy(stage[:, :, i * 128:(i + 1) * 128], tp[:, :, :]),
                     reads=[tpB], writes=[stageB])
            else:
                P.op("act", lambda e, tp=tp, stage=stage, i=i: e.copy(stage[:, :, i * 128:(i + 1) * 128], tp[:, :, :]),
                     reads=[tpB], writes=[stageB])
            n += 1
        P.dma("act", XSTv[:, :, g * 512:(g + 1) * 512], stage[:], reads=[stageB], writes=[dX], key=stageB)
    P.finish()


def phase_e(nc, G):
    P = Phase(nc, "e1")
    TT = 1024
    NH = TT // 512
    NS = TT // 128
    nff = DFE // 128
    XSTv = G["XST"].rearrange("(kc p) t -> p kc t", p=128)
    widx, widxB = load_const(P, [128, NT * NFC], I32, G["WIDX"])
    htr = Ring(P, [128, 8, TT], BF16, 2)
    aT, aTB = P.sb([128, nff, TT], BF16), Buf()
    w13r = Ring(P, [128, 2, 1024], BF16, 4)
    w2c = Ring(P, [128, 512], BF16, 6)
    sg = Ring(P, [128, 512], F32, 3)
    yor = Ring(P, [128, 512], F32, 4)
    psg = Ring(P, [128, 512], F32, 8, psum=True)
    dY = Buf(multi=True)
    WL = (G["moe_w1L"], G["moe_w3L"])
    W2h = (G["moe_w2h0"], G["moe_w2h1"])
    NROW = NE * DFE

    def gather(e, out, src, col):
        return e.indirect_dma_start(out=out, out_offset=None, in_=src,
                                    in_offset=bass.IndirectOffsetOnAxis(ap=widx[:, col:col + 1], axis=0),
                                    bounds_check=P.bcreg(e, NROW - 1), oob_is_err=False)

    ncopy = 0
    for j in range(NT):
        ts = slice(j * TT, (j + 1) * TT)
        ht, htB = htr.next()
        P.dma("sp", ht[:], XSTv[:, :, ts], writes=[htB], key=htB)
        for c in range(nff):
            w13, w13B = w13r.next()
            for which in range(2):
                P.dma("pool", None, None, reads=[widxB], writes=[w13B], key=w13B,
                      fn=lambda e, w13=w13, which=which, j=j, c=c: gather(e, w13[:, which, :], WL[which][:, :], j * NFC + c))
            for hh in range(NH):
                pg, pgB = psg.next()
                pu, puB = psg.next()
                hsl = slice(hh * 512, (hh + 1) * 512)

                def mmg(e, ps=pg, w13=w13, ht=ht, which=0, hsl=hsl):
                    for kc in range(8):
                        ins = e.matmul(ps[:], w13[:, which, kc * 128:(kc + 1) * 128], ht[:, kc, hsl], start=(kc == 0), stop=(kc == 7))
                    return ins
                P.op("pe", mmg, reads=[w13B, htB], writes=[pgB])
                P.op("pe", lambda e, pu=pu, w13=w13, ht=ht, hsl=hsl, mmg=mmg: mmg(e, pu, w13, ht, 1, hsl), reads=[w13B, htB], writes=[puB])
                s_, sB = sg.next()
                P.op("act", lambda e, s_=s_, pg=pg: e.activation(s_[:], pg[:], AF.Silu), reads=[pgB], writes=[sB])
                P.op("dve", lambda e, s_=s_, pu=pu, c=c, hsl=hsl: e.tensor_tensor(out=aT[:, c, hsl], in0=pu[:], in1=s_[:], op=ALU.mult),
                     reads=[puB, sB], writes=[aTB])
        for hf in range(2):
            pyl = [psg.next() for _ in range(NS)]
            for c in range(nff):
                w2t, w2B = w2c.next()
                P.dma("pool", None, None, reads=[widxB], writes=[w2B], key=w2B,
                      fn=lambda e, w2t=w2t, j=j, c=c, hf=hf: gather(e, w2t[:, :], W2h[hf][:, :], j * NFC + c))
                for i in range(NS):
                    py, pyB = pyl[i]
                    P.op("pe", lambda e, py=py, i=i, c=c, w2t=w2t: e.matmul(py[:], aT[:, c, i * 128:(i + 1) * 128], w2t[:, 0:512],
                                                                           start=(c == 0), stop=(c == nff - 1)),
                         reads=[aTB, w2B], writes=[pyB])
            for i in range(NS):
                py, pyB = pyl[i]
                yo, yoB = yor.next()
                if ncopy % 2 == 0:
                    P.op("dve", lambda e, yo=yo, py=py: e.tensor_copy(yo[:], py[:]), reads=[pyB], writes=[yoB])
                else:
                    P.op("act", lambda e, yo=yo, py=py: e.copy(yo[:], py[:]), reads=[pyB], writes=[yoB])
                ncopy += 1
                s0 = j * TT + i * 128
                P.dma("act", G["YS"][s0:s0 + 128, hf * 512:(hf + 1) * 512], yo[:], reads=[yoB], writes=[dY], key=yoB)
    P.finish()


def phase_c(nc, G, l):
    P = Phase(nc, "c1")
    NTI = T // 128
    C = LNCtx(P, G, G["ln_ffn_g"][l:l + 1, :], G["ln_ffn_b"][l:l + 1, :], want_T=False)
    sli, sliB = P.sb([128, NTI * 2], I32), Buf()
    P.dma("sp", sli[:].rearrange("p (i k) -> p i k", k=2), G["SLOT"].rearrange("(i p) k -> p i k", p=128), writes=[sliB], key=sliB)
    cws, cwsB = load_const(P, [128, NTI, 2], F32, G["CWS"].rearrange("(i p) k -> p i k", p=128))
    hin = Ring(P, [128, D], F32, 3)
    yar = Ring(P, [128, D], F32, 3)
    ybr = Ring(P, [128, D], F32, 3)
    accr = Ring(P, [128, D], F32, 3)
    hnr = Ring(P, [128, D], F32, 3)
    dH = Buf(multi=True)
    YS = G["YS"]
    for i in range(NTI):
        t0 = i * 128
        hi, hiB = hin.next()
        P.dma("sp", hi[:], G["Hsrc"][t0:t0 + 128, :], writes=[hiB], key=hiB)
        ys = []
        for k, ring in ((0, yar), (1, ybr)):
            y, yB = ring.next()
            P.dma("pool", None, None, reads=[sliB], writes=[yB], key=yB,
                  fn=lambda e, y=y, i=i, k=k: e.indirect_dma_start(
                      out=y[:, :], out_offset=None, in_=YS[:, :],
                      in_offset=bass.IndirectOffsetOnAxis(ap=sli[:, 2 * i + k:2 * i + k + 1], axis=0),
                      bounds_check=P.bcreg(e), oob_is_err=False))
            ys.append((y, yB))
        ac, acB = accr.next()
        P.op("act", lambda e, ac=ac, hi=hi: e.activation(ac[:], hi[:], AF.Copy, scale=float(ALPHA)), reads=[hiB], writes=[acB])
        for k in range(2):
            y, yB = ys[k]
            P.op("dve", lambda e, ac=ac, y=y, i=i, k=k: e.scalar_tensor_tensor(
                out=ac[:], in0=y[:], scalar=cws[:, i, k:k + 1], in1=ac[:], op0=ALU.mult, op1=ALU.add),
                reads=[yB, cwsB, acB], writes=[acB])
        hn, hnB = hnr.next()
        C.norm(ac, acB, hn, hnB)
        P.dma("act", G["out"][t0:t0 + 128, :], hn[:], reads=[hnB], writes=[dH], key=hnB)
    P.finish()

_CONST = {}


def host_consts():
    if _CONST:
        return _CONST
    bf = ml_dtypes.bfloat16
    s = np.arange(L, dtype=np.float64)
    f = np.arange(L, dtype=np.float64)
    ang = 2.0 * np.pi * np.outer(s, f) / NFFT
    Fm = np.concatenate([np.cos(ang), np.sin(ang)], axis=1)
    Fb = Fm.reshape(32, 128, 64, 128).transpose(2, 1, 0, 3)
    _CONST["dftF"] = np.ascontiguousarray(Fb).astype(bf)
    Gc = (2.0 / NFFT) * np.cos(ang.T)
    Gc[0, :] = 1.0 / NFFT
    Gs = (2.0 / NFFT) * np.sin(ang.T)
    Gm = np.concatenate([Gc, Gs], axis=0)
    Gb = Gm.reshape(4, 16, 128, 8, 512).transpose(3, 0, 2, 1, 4)
    _CONST["dftG"] = np.ascontiguousarray(Gb).astype(bf)
    sgn = np.where(np.arange(128) % 2 == 0, 1.0, -1.0)
    _CONST["nyqF"] = np.ascontiguousarray(np.repeat(sgn[:, None], 32, axis=1)).astype(bf)
    _CONST["nyqG"] = ((1.0 / NFFT) * np.where(np.arange(L) % 2 == 0, 1.0, -1.0))[None, :].astype(bf)
    t = np.linspace(0.0, 1.0, L, dtype=np.float32)[:, None]
    bands = np.linspace(1e-4, 16 - 1, 16, dtype=np.float32)[None, :]
    w = (np.float32(2.0 * math.pi / L) * np.arange(L, dtype=np.float32))[:, None]
    angp = bands * w
    z = np.concatenate([t, np.cos(angp), -np.sin(angp)], axis=-1).astype(np.float32)
    _CONST["posz"] = np.ascontiguousarray(z.T)
    _CONST["tcol"] = np.ascontiguousarray(t[:, 0].reshape(32, 128).T)
    max_decay = math.log(1e-2) / 0.3
    min_decay = math.log(1e-2) / 1.5
    deltas = np.linspace(min_decay, max_decay, D, dtype=np.float32)
    _CONST["negdelta"] = (-np.abs(deltas))[None, :].astype(np.float32)
    _CONST["identb"] = np.eye(128, dtype=np.float32).astype(bf)
    _CONST["identf"] = np.eye(128, dtype=np.float32)
    _CONST["ones"] = np.ones((128, 128), np.float32)
    _CONST["onesb"] = np.ones((128, 128), np.float32).astype(bf)
    _CONST["cidx"] = (np.arange(NFC, dtype=np.float32)[None, :] * 128 + np.arange(128, dtype=np.float32)[:, None])
    _CONST["triu"] = np.triu(np.ones((128, 128), np.float32), k=1).astype(bf)
    _CONST["eps"] = np.full((128, 1), LN_EPS, np.float32)
    _CONST["eps6"] = np.full((128, 1), 1e-6, np.float32)
    return _CONST


WEIGHT_NAMES = ["ln_in_g", "ln_in_b", "w_in", "flt_w1", "flt_w2", "flt_w3", "hyena_bias", "w_a_out", "w_h_out", "w_o",
                "ln_mix_g", "ln_mix_b", "ffn_w1", "ffn_w3", "ffn_w2", "moe_router", "moe_w1", "moe_w3", "moe_w2",
                "ln_ffn_g", "ln_ffn_b"]

SCRATCH = {
    "H": ([T, D], F32), "HT": ([D, T], BF16), "YA": ([D, T], BF16), "X0T": ([D, T], BF16), "GT": ([2 * D, T], BF16),
    "Z": ([NBC, 8, 128, L], BF16), "KRAW": ([L, 2 * D], F32), "KC": ([L, D], F32), "KS": ([L, D], F32), "KN": ([1, D], F32),
    "YH": ([D, T], BF16), "LG": ([T, NE], F32),
    "XS": ([NSLOT, D], BF16), "XST": ([D, NSLOT], BF16), "YS": ([NSLOT, D], F32), "ETILE": ([1, NT], I32),
    "SLOT": ([T, 2], I32), "CWS": ([T, 2], F32), "WIDX": ([128, NT * NFC], I32),
}

PHASES = ["ln0", "a0", "f0", "h0", "o0", "ffn0", "a1", "f1", "h1", "o1", "r1", "x1", "e1", "c1"]


class LazyG(dict):
    def __init__(self, nc, shapes, outs):
        super().__init__()
        self.nc, self.shapes, self.outs = nc, shapes, outs
        self.used_inputs = []

    def __missing__(self, name):
        nc = self.nc
        if name in ("Hsrc", "HTsrc", "LGsrc"):
            base = name[:-3]
            ap = self[base + "in"] if (base + "in") in self.shapes else self[base]
        elif name in self.shapes:
            shape, dt = self.shapes[name]
            bdt = BF16 if dt == ml_dtypes.bfloat16 else F32
            ap = nc.dram_tensor(name, list(shape), bdt, kind="ExternalInput").ap()
            self.used_inputs.append(name)
        elif name == "out":
            ap = nc.dram_tensor("out", [T, D], F32, kind="ExternalOutput").ap()
        else:
            shape, dt = SCRATCH[name]
            kind = "ExternalOutput" if name in self.outs else "Internal"
            ap = nc.dram_tensor(name, list(shape), dt, kind=kind).ap()
        self[name] = ap
        return ap


def build(shapes, phases=None, dbg_out=()):
    nc = bass.Bass("TRN2", target_bir_lowering=False)
    G = LazyG(nc, shapes, set(dbg_out))
    phases = PHASES if phases is None else phases
    for ph in phases:
        if ph == "ln0":
            phase_ln0(nc, G)
        elif ph[0] == "a":
            phase_a(nc, G, int(ph[1]))
        elif ph[0] == "f" and ph[1] != "f":
            phase_f(nc, G, int(ph[1]))
        elif ph[0] == "h":
            phase_h(nc, G, int(ph[1]))
        elif ph[0] == "o":
            phase_o(nc, G, int(ph[1]), want_logits=(ph[1] == "1"))
        elif ph == "r1":
            phase_r(nc, G)
        elif ph == "x1":
            phase_x(nc, G)
        elif ph == "e1":
            phase_e(nc, G)
        elif ph == "c1":
            phase_c(nc, G, 1)
        elif ph.startswith("ffn"):
            phase_ffn(nc, G, int(ph[3]), moe=(ph[3] == "1"))
        if ph == "ln0" or ph[0] == "o" or ph == "ffn0":
            G["Hsrc"] = G["H"]
            G["HTsrc"] = G["HT"]
        if ph == "o1":
            G["LGsrc"] = G["LG"]
    if "out" not in G and not dbg_out:
        pass
    return nc, list(G.used_inputs)


def host_inputs(inputs):
    rep = dict(host_consts())
    for k in WEIGHT_NAMES:
        a = np.asarray(inputs[k], dtype=np.float32)
        if a.ndim == 1:
            a = a[None, :]
        rep[k] = np.ascontiguousarray(a)
    ca = np.asarray(inputs["conv_a_w"], np.float32)
    chw = np.asarray(inputs["conv_h_w"], np.float32)
    chb = np.asarray(inputs["conv_h_b"], np.float32)
    cols = [ca[:, j, :] for j in range(3)]
    for k in range(3):
        cols += [chw[:, j, k * D:(k + 1) * D] for j in range(3)]
    cols += [chb[:, k * D:(k + 1) * D] for k in range(3)]
    cols += [np.zeros_like(cols[0])]
    prm = np.stack(cols, axis=-1)
    rep["cprm"] = np.ascontiguousarray(prm.reshape(2, 8, 128, 16))
    for nm in ("moe_w1", "moe_w3"):
        w = rep.pop(nm)[0]
        rep[nm + "L"] = np.ascontiguousarray(w.reshape(NE, 8, 128, NFC, 128).transpose(0, 3, 2, 1, 4)).reshape(NE * DFE, 1024)
    w2 = rep.pop("moe_w2")[0].reshape(NE * DFE, 2, 512)
    rep["moe_w2h0"] = np.ascontiguousarray(w2[:, 0, :])
    rep["moe_w2h1"] = np.ascontiguousarray(w2[:, 1, :])
    fq = np.asarray(inputs["flt_freq"], np.float32)
    rep["fprm"] = np.ascontiguousarray(np.stack([np.asarray(inputs["flt_b1"], np.float32), fq,
                                                 np.asarray(inputs["flt_b2"], np.float32), fq], axis=-1))
    return rep


LAUNCHES = [PHASES]
HANDOVER = {"H": "Hin", "HT": "HTin", "LG": "LGin"}


def kernel(**inputs):
    x = np.asarray(inputs["x"], dtype=np.float32)
    rep = host_inputs(inputs)
    xs = x.reshape(NCORES, T, D)
    carry = [dict() for _ in range(NCORES)]
    res = None
    for li, phases in enumerate(LAUNCHES):
        shapes = {k: (v.shape, v.dtype) for k, v in rep.items()}
        shapes["x"] = ((T, D), np.float32)
        for k, v in carry[0].items():
            shapes[k] = (v.shape, v.dtype)
        last = li == len(LAUNCHES) - 1
        outs = () if last else (("H", "HT", "LG") if "o1" in phases else ("H", "HT"))
        nc, used = build(shapes, phases=phases, dbg_out=outs)
        in_maps = []
        for c in range(NCORES):
            m = {}
            for k in used:
                if k == "x":
                    m[k] = np.ascontiguousarray(xs[c])
                elif k in carry[c]:
                    m[k] = carry[c][k]
                else:
                    m[k] = rep[k]
            in_maps.append(m)
        res = run_bass_kernel_spmd(nc, in_maps, core_ids=list(range(NCORES)))
        if not last:
            for c in range(NCORES):
                for k in outs:
                    carry[c][HANDOVER[k]] = np.asarray(res.results[c][k])
    out = np.stack([np.asarray(r["out"], dtype=np.float32) for r in res.results], axis=0)
    return out.reshape(16, L, D)
```
